# Optimizing a Trainium2 kernel written in Bass

```python
import jax, jax.numpy as jnp
from jax import lax
import numpy as np

D_MODEL = 1024
BATCH = 4
SEQ = 8192
DEPTH = 2

HGRN_HEADS = 8
HGRN_EXPAND = 128
HGRN_VDIM = 128
HGRN_KEY_WIDTH = HGRN_HEADS * HGRN_EXPAND
HGRN_VAL_WIDTH = HGRN_HEADS * HGRN_VDIM
HGRN_SCALE = HGRN_EXPAND ** -0.5
CHUNK = 64
F_MIN = 1e-6
CONV_CH = D_MODEL
CONV_K = 3
D_FF = 2816
N_EXPERTS = 8
TOP_K = 2
EPS = 1e-6
N_DENSE = (DEPTH + 1) // 2
N_MOE = DEPTH // 2
IN_SPLITS = (HGRN_KEY_WIDTH, HGRN_KEY_WIDTH, HGRN_VAL_WIDTH, HGRN_VAL_WIDTH,
             CONV_CH, CONV_CH, CONV_CH, D_MODEL, D_MODEL)
IN_WIDTH = sum(IN_SPLITS)

kernel_name = "hgrn2_shortconv_gated_merge_moe"


def rms_norm(x, w):
    xf = x.astype(jnp.float32)
    y = xf * lax.rsqrt(jnp.mean(xf * xf, axis=-1, keepdims=True) + EPS)
    return (y * w.astype(jnp.float32)).astype(x.dtype)


def split_projection(h, w):
    outs = []
    start = 0
    for width in IN_SPLITS:
        outs.append(jnp.einsum('bsd,de->bse', h, w[:, start:start + width]))
        start += width
    return outs


def hgrn2_chunk_recurrence(q, k, v, log_f):
    bsz, seq, nh, kd = q.shape
    vd = v.shape[-1]
    n_chunks = seq // CHUNK

    def to_chunks(t):
        return t.reshape(bsz, n_chunks, CHUNK, nh, t.shape[-1]).transpose(1, 0, 3, 2, 4)

    causal = jnp.tril(jnp.ones((CHUNK, CHUNK), dtype=bool))[:, :, None]

    def step(state, inp):
        qc, kc, vc, gc = inp
        cum = jnp.cumsum(gc, axis=2)
        diff = cum[:, :, :, None, :] - cum[:, :, None, :, :]
        decay = jnp.where(causal, jnp.exp(jnp.where(causal, diff, 0.0)), 0.0)
        scores = jnp.einsum('bhtk,bhtsk,bhsk->bhts', qc, decay, kc)
        o = (jnp.einsum('bhts,bhsv->bhtv', scores, vc)
             + jnp.einsum('bhtk,bhkv->bhtv', qc * jnp.exp(cum), state))
        last = cum[:, :, -1:, :]
        state = (jnp.exp(last[:, :, 0, :])[..., None] * state
                 + jnp.einsum('bhsk,bhsv->bhkv', kc * jnp.exp(last - cum), vc))
        return state, o

    s0 = jnp.zeros((bsz, nh, kd, vd), jnp.float32)
    _, o = lax.scan(step, s0, (to_chunks(q), to_chunks(k), to_chunks(v), to_chunks(log_f)))
    return o.transpose(1, 0, 3, 2, 4).reshape(bsz, seq, nh, vd)


def hybrid_mixer(h, w_in, lb, g_norm_w, conv_w, w_proj_hgrn, w_proj_conv, w_out):
    bsz, seq, _ = h.shape
    q, f_logit, i_in, g_out, b_gate, c_gate, u, gate_a, gate_b = split_projection(h, w_in)

    def heads(t):
        return t.reshape(bsz, seq, HGRN_HEADS, -1).astype(jnp.float32)

    z = heads(f_logit)
    lbh = lb.astype(jnp.float32).reshape(HGRN_HEADS, HGRN_EXPAND)
    f = lbh + (1.0 - lbh) * jax.nn.sigmoid(z)
    log_f = jnp.log(jnp.maximum(f, F_MIN))
    k = (1.0 - lbh) * jax.nn.sigmoid(-z)
    qh = jax.nn.silu(heads(q)) * HGRN_SCALE
    o = hgrn2_chunk_recurrence(qh, k, heads(i_in), log_f)
    o = rms_norm(o, g_norm_w) * jax.nn.silu(heads(g_out))
    y_a = jnp.einsum('bse,ed->bsd', o.reshape(bsz, seq, HGRN_VAL_WIDTH).astype(h.dtype), w_proj_hgrn)

    v = b_gate * u
    vp = jnp.pad(v, ((0, 0), (CONV_K - 1, 0), (0, 0)))
    conv = conv_w[0] * vp[:, 0:seq, :]
    for j in range(1, CONV_K):
        conv = conv + conv_w[j] * vp[:, j:j + seq, :]
    y_b = jnp.einsum('bse,ed->bsd', c_gate * conv, w_proj_conv)

    merged = jax.nn.sigmoid(gate_a) * y_a + jax.nn.sigmoid(gate_b) * y_b
    return jnp.einsum('bsd,de->bse', merged, w_out)


def swiglu(h, w1, w3, w2):
    a = jnp.einsum('bsd,df->bsf', h, w1)
    b = jnp.einsum('bsd,df->bsf', h, w3)
    return jnp.einsum('bsf,fd->bsd', jax.nn.silu(a) * b, w2)


def moe_swiglu(h, router_w, w1, w3, w2):
    bsz, seq, d = h.shape
    t = h.reshape(bsz * seq, d)
    logits = jnp.einsum('td,de->te', t, router_w).astype(jnp.float32)
    top_vals, top_idx = lax.top_k(logits, TOP_K)
    top_w = jax.nn.softmax(top_vals, axis=-1)
    gates = jnp.sum(jax.nn.one_hot(top_idx, N_EXPERTS, dtype=jnp.float32) * top_w[..., None], axis=1)
    gates = gates.astype(h.dtype)
    out = jnp.zeros_like(t)
    for e in range(N_EXPERTS):
        a = t @ w1[e]
        b = t @ w3[e]
        out = out + gates[:, e:e + 1] * ((jax.nn.silu(a) * b) @ w2[e])
    return out.reshape(bsz, seq, d)


def setup_inputs(seed: int = 0) -> dict:
    key = jax.random.key(seed)
    ks = jax.random.split(key, 20)
    nrm = lambda k, shape, fan_in: jax.random.normal(k, shape, jnp.float32) * (fan_in ** -0.5)
    gain = lambda k, shape: 1.0 + 0.05 * jax.random.normal(k, shape, jnp.float32)
    return {
        "x": jax.random.normal(ks[0], (BATCH, SEQ, D_MODEL), jnp.float32),
        "w_in": nrm(ks[1], (DEPTH, D_MODEL, IN_WIDTH), D_MODEL),
        "lower_bounds": 0.1 * jax.random.normal(ks[2], (DEPTH, HGRN_KEY_WIDTH), jnp.float32),
        "hgrn_norm_w": gain(ks[3], (DEPTH, HGRN_VDIM)),
        "conv_w": nrm(ks[4], (DEPTH, CONV_K, CONV_CH), CONV_K),
        "w_proj_hgrn": nrm(ks[5], (DEPTH, HGRN_VAL_WIDTH, D_MODEL), HGRN_VAL_WIDTH),
        "w_proj_conv": nrm(ks[6], (DEPTH, CONV_CH, D_MODEL), CONV_CH),
        "w_out": nrm(ks[7], (DEPTH, D_MODEL, D_MODEL), D_MODEL),
        "norm_mix": gain(ks[8], (DEPTH, D_MODEL)),
        "norm_ffn": gain(ks[9], (DEPTH, D_MODEL)),
        "dense_w1": nrm(ks[10], (N_DENSE, D_MODEL, D_FF), D_MODEL),
        "dense_w3": nrm(ks[11], (N_DENSE, D_MODEL, D_FF), D_MODEL),
        "dense_w2": nrm(ks[12], (N_DENSE, D_FF, D_MODEL), D_FF),
        "router_w": nrm(ks[13], (N_MOE, D_MODEL, N_EXPERTS), D_MODEL),
        "expert_w1": nrm(ks[14], (N_MOE, N_EXPERTS, D_MODEL, D_FF), D_MODEL),
        "expert_w3": nrm(ks[15], (N_MOE, N_EXPERTS, D_MODEL, D_FF), D_MODEL),
        "expert_w2": nrm(ks[16], (N_MOE, N_EXPERTS, D_FF, D_MODEL), D_FF),
        "final_norm": gain(ks[17], (D_MODEL,)),
    }


def reference(x, w_in, lower_bounds, hgrn_norm_w, conv_w, w_proj_hgrn, w_proj_conv, w_out,
              norm_mix, norm_ffn, dense_w1, dense_w3, dense_w2, router_w,
              expert_w1, expert_w3, expert_w2, final_norm):
    lb_soft = jax.nn.softmax(lower_bounds.astype(jnp.float32), axis=0)
    lb_all = jnp.cumsum(lb_soft, axis=0) - lb_soft[0]
    for layer in range(DEPTH):
        h = rms_norm(x, norm_mix[layer])
        x = x + hybrid_mixer(h, w_in[layer], lb_all[layer], hgrn_norm_w[layer], conv_w[layer],
                             w_proj_hgrn[layer], w_proj_conv[layer], w_out[layer])
        h = rms_norm(x, norm_ffn[layer])
        if layer % 2 == 0:
            j = layer // 2
            x = x + swiglu(h, dense_w1[j], dense_w3[j], dense_w2[j])
        else:
            j = layer // 2
            x = x + moe_swiglu(h, router_w[j], expert_w1[j], expert_w3[j], expert_w2[j])
    return rms_norm(x, final_norm)
```

```python
import numpy as np
from contextlib import ExitStack
import concourse.bass as bass
import concourse.mybir as mybir
from concourse.bass_utils import run_bass_kernel_spmd

F32 = mybir.dt.float32
BF16 = mybir.dt.bfloat16
AF = mybir.ActivationFunctionType
ALU = mybir.AluOpType
AX = mybir.AxisListType

D = 1024
NH = 8
DFF = 2816
NE = 8
DEPTH = 2
INW = 9216
EPS = 1e-6
F_MIN = 1e-6
SCALE = 128 ** -0.5
NCORES = 8


class _Rec:
    def __init__(self):
        self.call = None

    def __getattr__(self, name):
        def f(*a, **k):
            self.call = (name, a, k)
        return f


class Prog:
    ENG = ("pe", "act", "dve", "pool", "sp")

    def __init__(self, nc):
        self.nc = nc
        self.ops = []
        self.last_w = {}
        self.readers = {}
        self.last_on = {}
        self.asyncs = []

    def add(self, eng, fn, reads=(), writes=(), ainc=0, semkey=None):
        i = len(self.ops)
        deps = set()
        for k in reads:
            if k in self.last_w:
                deps.add(self.last_w[k])
        for k in writes:
            if k in self.last_w:
                deps.add(self.last_w[k])
            deps.update(self.readers.get(k, ()))
        for k in reads:
            self.readers.setdefault(k, []).append(i)
        for k in writes:
            self.last_w[k] = i
            self.readers[k] = []
        if fn is not None:
            rec = _Rec()
            fn(rec)
            name_, a_, k_ = rec.call
            fn = (lambda e, name_=name_, a_=a_, k_=k_: getattr(e, name_)(*a_, **k_))
        self.ops.append(dict(eng=eng, fn=fn, deps=deps, ainc=ainc, semkey=semkey, signal=False))
        if ainc:
            self.asyncs.append(i)
        elif fn is not None:
            self.last_on[eng] = i
        return i

    def dma(self, eng, out, in_, reads, writes, semkey, **kw):
        return self.add(eng, lambda e: e.dma_start(out=out, in_=in_, **kw), reads, writes,
                        ainc=16, semkey=(eng, semkey))

    def barrier(self):
        deps = set(self.last_on.values()) | set(self.asyncs)
        for e in self.ENG:
            i = self.add(e, None)
            self.ops[i]["deps"] = set(deps)
        self.asyncs = []

    @staticmethod
    def _skip(od, o):
        return (not od["ainc"]) and od["eng"] == "pe" and o["eng"] == "pe" and not o["ainc"] \
            and o["fn"] is not None

    def emit(self, final_wait_eng="sp"):
        nc = self.nc
        ops = self.ops
        for o in ops:
            for d in o["deps"]:
                od = ops[d]
                if od["ainc"] or self._skip(od, o):
                    continue
                od["signal"] = True
        eng_cnt = {e: 0 for e in self.ENG}
        a_cnt = {}
        for o in ops:
            if o["ainc"]:
                a_cnt[o["semkey"]] = a_cnt.get(o["semkey"], 0) + o["ainc"]
                o["cnt"] = a_cnt[o["semkey"]]
            elif o["signal"]:
                eng_cnt[o["eng"]] += 1
                o["cnt"] = eng_cnt[o["eng"]]
        running = {}
        for o in ops:
            waits = {}
            for d in o["deps"]:
                od = ops[d]
                if od["ainc"]:
                    key = ("a", od["semkey"])
                    val = running[od["semkey"]]
                else:
                    if self._skip(od, o):
                        continue
                    key = ("e", od["eng"])
                    val = od["cnt"]
                waits[key] = max(waits.get(key, 0), val)
            o["waits"] = waits
            if o["ainc"]:
                running[o["semkey"]] = o["cnt"]
        seen = {e: {} for e in self.ENG}
        for o in ops:
            s = seen[o["eng"]]
            w2 = {}
            for k, v in o["waits"].items():
                if s.get(k, 0) >= v:
                    continue
                w2[k] = v
                s[k] = v
            o["waits"] = w2
        with ExitStack() as st:
            esem = {e: st.enter_context(nc.semaphore("s_" + e)) for e in self.ENG}
            asem = {k: st.enter_context(nc.semaphore("a_%d" % j)) for j, k in enumerate(a_cnt)}
            block = st.enter_context(nc.Block())

            def run_engine(ename):
                def body(e):
                    for o in ops:
                        if o["eng"] != ename:
                            continue
                        for (kind, k), v in o["waits"].items():
                            e.wait_ge(esem[k] if kind == "e" else asem[k], v)
                        if o["fn"] is None:
                            continue
                        ins = o["fn"](e)
                        if o["ainc"]:
                            ins.then_inc(asem[o["semkey"]], o["ainc"])
                        elif o["signal"]:
                            ins.then_inc(esem[ename], 1)
                    if ename == final_wait_eng:
                        for k, v in a_cnt.items():
                            e.wait_ge(asem[k], v)
                return body

            block.tensor(run_engine("pe"))
            block.scalar(run_engine("act"))
            block.vector(run_engine("dve"))
            block.gpsimd(run_engine("pool"))
            block.sync(run_engine("sp"))
        return len(a_cnt) + len(self.ENG)


class _Stop(Exception):
    pass


def build_program(NT, dbg=None, stop_after=None, lite=False, a1cut=99):
    nc = bass.Bass("TRN2", target_bir_lowering=False)
    NT128 = NT // 128
    NT256 = NT // 256
    NT512 = NT // 512
    NCH = NT // 64
    HALF = min(2048, NT)
    NHALF = NT // HALF

    def din(name, shape, dt=F32):
        if lite and name == "w_in":
            shape = [1, D, 4096]
        elif lite and name in ("w_proj_hgrn", "w_proj_conv", "w_out"):
            shape = [1, 128, 128]
        elif lite and name in ("dense_blob", "expert_blob"):
            shape = [1, 1, 128, 128]
        return nc.dram_tensor(name, list(shape), dt, kind="ExternalInput").ap()

    def dscr(name, shape, dt=F32):
        return nc.dram_tensor(name, list(shape), dt, kind="Internal").ap()

    x_in = din("x", [NT, D])
    w_in = din("w_in", [DEPTH, D, INW])
    lbnd = din("lower_bounds", [DEPTH, D])
    gnw_in = din("hgrn_norm_w", [DEPTH, 128, 1])
    convw_in = din("conv_w", [DEPTH, 128, 24])
    wph_in = din("w_proj_hgrn", [DEPTH, D, D])
    wpc_in = din("w_proj_conv", [DEPTH, D, D])
    wo_in = din("w_out", [DEPTH, D, D])
    nmix_in = din("norm_mix", [DEPTH, 128, 8])
    nffn_in = din("norm_ffn", [DEPTH, 128, 8])
    dblob_in = din("dense_blob", [1, DFF // 256, 128, 6144])
    rw_in = din("router_w", [128, 8, NE])
    eblob_in = din("expert_blob", [NE, DFF // 256, 128, 6144])
    fnorm_in = din("final_norm", [128, 8])
    c_ident = din("c_ident", [128, 128])
    c_triu = din("c_triu", [128, 128])
    c_tris = din("c_tris", [128, 128])
    c_mask = din("c_mask", [128, 8 * 64])
    c_sel = din("c_sel", [128, 1])
    out_d = nc.dram_tensor("out", [NT, D], F32, kind="ExternalOutput").ap()

    xT = dscr("xT", [8, 128, NT])
    qtT = dscr("qtT", [NT128, 128, 1024], BF16)
    ktT = dscr("ktT", [NT128, 128, 1024], BF16)
    khD = dscr("khD", [NT128, 128, 1024], BF16)
    vtD = dscr("vtD", [NT128, 128, 1024], BF16)
    gsT = dscr("gsT", [NT128, 128, 1024], BF16)
    opT = dscr("opT", [NT128, 128, 1024], F32)
    qbT = dscr("qbT", [NT128, 128, 1024], BF16)
    vcT = dscr("vcT", [8, 128, NT + 2], F32)
    ccT = dscr("ccT", [NT256, 128, 8 * 256], BF16)
    gaT = dscr("gaT", [NT256, 128, 8 * 256], BF16)
    gbT = dscr("gbT", [NT256, 128, 8 * 256], BF16)
    gin = dscr("gin", [1040, 128])
    gout = dscr("gout", [2080, 128])

    dbg_outs = {}
    if dbg:
        for name, shape in dbg.items():
            dbg_outs[name] = nc.dram_tensor("dbg_" + name, list(shape), F32, kind="ExternalOutput").ap()

    P = Prog(nc)
    with ExitStack() as gst:
        uniq = [0]

        def sb(name, shape, dt=F32, st=gst):
            uniq[0] += 1
            return st.enter_context(nc.sbuf_tensor("%s_%d" % (name, uniq[0]), list(shape), dt))

        ident = sb("ident", [128, 128])
        ident_bf = sb("ident_bf", [128, 128], BF16)
        ones = sb("ones", [128, 128])
        triu = sb("triu", [128, 128])
        tris = sb("tris", [128, 128])
        amask = sb("amask", [128, 8, 64])
        sel = sb("sel", [128, 1])
        gnw = sb("gnw", [128, DEPTH])
        convw = sb("convw", [128, DEPTH, 24])
        nmix = sb("nmix", [128, DEPTH, 8])
        nffn = sb("nffn", [128, DEPTH, 8])
        fnw = sb("fnw", [128, 8])
        rw = sb("rw", [128, 8, NE])
        totT = sb("totT", [128, 8, NCH])
        epsb = sb("epsb", [128, 1])
        PS = [gst.enter_context(nc.psum_tensor("ps%d" % i, [128, 1024], F32)) for i in range(4)]
        psrr = [0]

        def nps():
            i = psrr[0] % 4
            psrr[0] += 1
            return PS[i], "ps%d" % i

        C = "const"
        P.dma("sp", ident[:], c_ident[:, :], [], [C], "c0")
        P.dma("sp", triu[:], c_triu[:, :], [], [C], "c0")
        P.dma("sp", tris[:], c_tris[:, :], [], [C], "c0")
        P.dma("sp", amask[:].rearrange("p h t -> p (h t)"), c_mask[:, :], [], [C], "c0")
        P.dma("sp", sel[:], c_sel[:, :], [], [C], "c0")
        P.dma("sp", fnw[:], fnorm_in[:, :], [], [C], "c0")
        P.dma("sp", rw[:], rw_in[:, :, :], [], [C], "c0")
        for l in range(DEPTH):
            P.dma("sp", gnw[:, l:l + 1], gnw_in[l, :, :], [], [C], "c0")
            P.dma("sp", convw[:, l, :], convw_in[l, :, :], [], [C], "c0")
            P.dma("sp", nmix[:, l, :], nmix_in[l, :, :], [], [C], "c0")
            P.dma("sp", nffn[:, l, :], nffn_in[l, :, :], [], [C], "c0")
        P.add("pool", lambda e: e.memset(ones[:], 1.0), [], ["ones"])
        P.add("pool", lambda e: e.memset(epsb[:], EPS), [], ["epsb"])
        P.add("dve", lambda e: e.tensor_copy(out=ident_bf[:], in_=ident[:]), [C], ["identbf"])
        P.barrier()

        def chk(name):
            if stop_after == name:
                raise _Stop()

        try:
            with ExitStack() as st:
                xin = [sb("p0_x%d" % i, [128, 1024], st=st) for i in range(4)]
                xo = [sb("p0_o%d" % i, [128, 8, 128], st=st) for i in range(4)]
                for i in range(NT128):
                    b = i % 4
                    P.dma("sp", xin[b][:], x_in[i * 128:(i + 1) * 128, :], [], ["p0x%d" % b], "p0x%d" % b)
                    ps, pk = nps()
                    for dc in range(8):
                        P.add("pe", lambda e, ps=ps, b=b, dc=dc: e.transpose(
                            out=ps[:, dc * 128:(dc + 1) * 128], in_=xin[b][:, dc * 128:(dc + 1) * 128],
                            identity=ident[:]), ["p0x%d" % b, C], [pk])
                    P.add("dve", lambda e, ps=ps, b=b: e.tensor_copy(
                        out=xo[b][:].rearrange("p c t -> p (c t)"), in_=ps[:, :]), [pk], ["p0o%d" % b])
                    P.dma("pool", xT[:, :, i * 128:(i + 1) * 128].rearrange("c p t -> p c t"), xo[b][:],
                          ["p0o%d" % b], ["xT"], "p0o%d" % b)
            P.barrier()
            chk("p0")

            def emit_norm(xt, xk, w_ap, hout, hk, sq, sqk, rstd, rk, n=512, h32=None, h32k=None):
                P.add("act", lambda e: e.activation(out=sq, in_=xt, func=AF.Square), [xk], [sqk])
                ps, pk = nps()
                for dc in range(8):
                    P.add("pe", lambda e, dc=dc: e.matmul(ps[:, 0:n], lhsT=ones[:], rhs=sq[:, dc, :],
                                                          start=(dc == 0), stop=(dc == 7)),
                          [sqk, "ones"], [pk])
                P.add("act", lambda e: e.activation(out=rstd, in_=ps[:, 0:n], func=AF.Ln,
                                                    scale=1.0 / D, bias=epsb[:]), [pk, "epsb"], [rk])
                P.add("act", lambda e: e.activation(out=rstd, in_=rstd, func=AF.Exp, scale=-0.5), [rk], [rk])
                for dc in range(8):
                    tgt = hout if h32 is None else h32
                    P.add("dve", lambda e, dc=dc, tgt=tgt: e.scalar_tensor_tensor(
                        out=tgt[:, dc, :], in0=xt[:, dc, :], scalar=w_ap(dc), in1=rstd,
                        op0=ALU.mult, op1=ALU.mult), [xk, rk, C], [hk if h32 is None else h32k])
                if h32 is not None:
                    P.add("pool", lambda e: e.tensor_copy(out=hout, in_=h32), [h32k], [hk])

            def load_w_bf16(dst, src, key, nk, ncols):
                for kc in range(nk):
                    P.dma("pool", dst[:, kc, :], src[kc * 128:(kc + 1) * 128, :], [], [key], key)

            for l in range(DEPTH):
                with ExitStack() as st:
                    wA = sb("a1_w", [128, 8, 4096], BF16, st=st)
                    load_w_bf16(wA, w_in[l, :, 0:4096], "a1w", 8, 4096)
                    xt = [sb("a1_xt%d" % i, [128, 8, 512], st=st) for i in range(2)]
                    sq = sb("a1_sq", [128, 8, 512], st=st)
                    rstd = sb("a1_rstd", [128, 512], st=st)
                    hT2 = [sb("a1_hT%d" % i, [128, 8, 512], BF16, st=st) for i in range(2)]
                    f_sb = sb("a1_f", [128, 1024], st=st)
                    lbt_l = sb("a1_lbt", [128, 1024], st=st)
                    lbm_l = sb("a1_lbm", [128, 1024], st=st)
                    if l == 0:
                        P.add("dve", lambda e: e.memset(lbt_l[:], 0.0), [], ["a1lb"])
                        P.add("dve", lambda e: e.memset(lbm_l[:], 1.0), [], ["a1lb"])
                    else:
                        P.dma("sp", lbt_l[:], bass.AP(lbnd.tensor, l * D, [[0, 128], [1, D]]), [], ["a1lb"], "a1lb")
                        P.dma("sp", lbm_l[:], bass.AP(lbnd.tensor, 0, [[0, 128], [1, D]]), [], ["a1lb"], "a1lb")
                        P.add("dve", lambda e: e.tensor_sub(out=lbt_l[:], in0=lbt_l[:], in1=lbm_l[:]), ["a1lb"], ["a1lb"])
                        P.add("act", lambda e: e.activation(out=lbt_l[:], in_=lbt_l[:], func=AF.Sigmoid), ["a1lb"], ["a1lb"])
                        P.add("dve", lambda e: e.tensor_scalar(out=lbm_l[:], in0=lbt_l[:], scalar1=-1.0, scalar2=1.0,
                                                               op0=ALU.mult, op1=ALU.add), ["a1lb"], ["a1lb"])
                    k_sb = sb("a1_k", [128, 1024], st=st)
                    g_sb = sb("a1_g", [128, 1024], st=st)
                    e_sb = sb("a1_e", [128, 1024], st=st)
                    kt_sb = [sb("a1_kt%d" % i, [128, 1024], st=st) for i in range(2)]
                    e2_sb = sb("a1_e2", [128, 1024], st=st)
                    pending_tp = [None]
                    kh_sb = [sb("a1_kh%d" % i, [128, 1024], BF16, st=st) for i in range(2)]
                    ktT_sb = [sb("a1_ktT%d" % i, [128, 1024], BF16, st=st) for i in range(2)]
                    qt_sb = [sb("a1_qt%d" % i, [128, 1024], BF16, st=st) for i in range(2)]
                    vt_sb = [sb("a1_vt%d" % i, [128, 1024], BF16, st=st) for i in range(2)]
                    gs_sb = [sb("a1_gs%d" % i, [128, 1024], BF16, st=st) for i in range(2)]
                    ecT = sb("a1_ecT", [128, 1024], st=st)
                    ct_sb = sb("a1_ct", [128, 1024], st=st)
                    sq_q = sb("a1_sqq", [128, 1024], st=st)

                    def ld_x(j):
                        b = j % 2
                        P.dma("sp", xt[b][:], xT[:, :, j * 512:(j + 1) * 512].rearrange("c p t -> p c t"),
                              ["xT"], ["a1xt%d" % b], "a1xt%d" % b)
                    ld_x(0)
                    if NT512 > 1:
                        ld_x(1)

                    def norm1(j):
                        hb = j % 2
                        emit_norm(xt[hb][:], "a1xt%d" % hb, lambda dc: nmix[:, l, dc:dc + 1], hT2[hb][:], "a1hT%d" % hb,
                                  sq[:], "a1sq", rstd[:], "a1rstd")
                    norm1(0)
                    for j in range(NT512):
                        b = j % 2
                        hT = hT2[b]
                        hk = "a1hT%d" % b
                        for s in range(4):
                            if s == 2:
                                if j + 1 < NT512:
                                    norm1(j + 1)
                                if j + 2 < NT512:
                                    ld_x(j + 2)
                            i = j * 4 + s
                            ob = i % 2
                            hs = hT[:, :, s * 128:(s + 1) * 128]
                            kb = i % 2
                            zp, zk = nps()
                            for hf in range(2):
                                for kc in range(8):
                                    P.add("pe", lambda e, hf=hf, kc=kc, zp=zp, hs=hs: e.matmul(
                                        zp[:, hf * 512:(hf + 1) * 512], lhsT=hs[:, kc, :],
                                        rhs=wA[:, kc, 1024 + hf * 512:1024 + (hf + 1) * 512],
                                        start=(kc == 0), stop=(kc == 7)), [hk, "a1w"], [zk])
                            P.add("act", lambda e, zp=zp: e.activation(out=f_sb[:], in_=zp[:, :], func=AF.Sigmoid),
                                  [zk], ["a1f"])
                            P.add("dve", lambda e: e.tensor_mul(out=f_sb[:], in0=f_sb[:], in1=lbm_l[:]),
                                  ["a1f", "a1lb"], ["a1f"])
                            P.add("dve", lambda e: e.tensor_add(out=f_sb[:], in0=f_sb[:], in1=lbt_l[:]),
                                  ["a1f", "a1lb"], ["a1f"])
                            P.add("dve", lambda e: e.tensor_scalar(out=k_sb[:], in0=f_sb[:], scalar1=-1.0, scalar2=1.0,
                                                                   op0=ALU.mult, op1=ALU.add), ["a1f"], ["a1k"])
                            P.add("dve", lambda e: e.tensor_scalar_max(out=f_sb[:], in0=f_sb[:], scalar1=F_MIN),
                                  ["a1f"], ["a1f"])
                            qp, qk = nps()
                            for h in range(8):
                                for kc in range(8):
                                    P.add("pe", lambda e, h=h, kc=kc, qp=qp, hs=hs: e.matmul(
                                        qp[:, h * 128:(h + 1) * 128], lhsT=wA[:, kc, h * 128:(h + 1) * 128],
                                        rhs=hs[:, kc, :], start=(kc == 0), stop=(kc == 7)), [hk, "a1w"], [qk])
                            P.add("act", lambda e, qp=qp: e.activation(out=sq_q[:], in_=qp[:, :], func=AF.Silu),
                                  [qk], ["a1sqq"])
                            if pending_tp[0] is not None:
                                pending_tp[0]()
                                pending_tp[0] = None
                            gp, gk = nps()
                            for h in range(8):
                                for kc in range(8):
                                    P.add("pe", lambda e, h=h, kc=kc, gp=gp, hs=hs: e.matmul(
                                        gp[:, h * 128:(h + 1) * 128], lhsT=wA[:, kc, 3072 + h * 128:3072 + (h + 1) * 128],
                                        rhs=hs[:, kc, :], start=(kc == 0), stop=(kc == 7)), [hk, "a1w"], [gk])
                            P.add("act", lambda e, gp=gp, ob=ob: e.activation(out=gs_sb[ob][:], in_=gp[:, :], func=AF.Silu),
                                  [gk], ["a1gs%d" % ob])
                            P.dma("pool", gsT[i, :, :], gs_sb[ob][:], ["a1gs%d" % ob], ["gsT"], "a1gs%d" % ob)
                            vp, vk = nps()
                            for hf in range(2):
                                for kc in range(8):
                                    P.add("pe", lambda e, hf=hf, kc=kc, vp=vp, hs=hs: e.matmul(
                                        vp[:, hf * 512:(hf + 1) * 512], lhsT=hs[:, kc, :],
                                        rhs=wA[:, kc, 2048 + hf * 512:2048 + (hf + 1) * 512],
                                        start=(kc == 0), stop=(kc == 7)), [hk, "a1w"], [vk])
                            P.add("act", lambda e, vp=vp, ob=ob: e.copy(out=vt_sb[ob][:], in_=vp[:, :]),
                                  [vk], ["a1vt%d" % ob])
                            P.dma("pool", vtD[i, :, :], vt_sb[ob][:], ["a1vt%d" % ob], ["vtD"], "a1vt%d" % ob)
                            P.add("act", lambda e: e.activation(out=g_sb[:], in_=f_sb[:], func=AF.Ln), ["a1f"], ["a1g"])
                            cp, ck = nps()
                            npz, nk_ = nps()
                            for hf in range(2):
                                P.add("pe", lambda e, hf=hf, cp=cp: e.matmul(
                                    cp[:, hf * 512:(hf + 1) * 512], lhsT=triu[:], rhs=g_sb[:, hf * 512:(hf + 1) * 512],
                                    start=True, stop=True), ["a1g", C], [ck])
                                P.add("pe", lambda e, hf=hf, npz=npz: e.matmul(
                                    npz[:, hf * 512:(hf + 1) * 512], lhsT=tris[:], rhs=g_sb[:, hf * 512:(hf + 1) * 512],
                                    start=True, stop=True), ["a1g", C], [nk_])
                            ctp, ctk = nps()
                            for h in range(8):
                                P.add("pe", lambda e, h=h, ctp=ctp: e.matmul(
                                    ctp[:, h * 128:(h + 1) * 128], lhsT=g_sb[:, h * 128:(h + 1) * 128], rhs=triu[:],
                                    start=True, stop=True), ["a1g", C], [ctk])
                            P.add("act", lambda e, cp=cp: e.activation(out=e_sb[:], in_=cp[:, :], func=AF.Exp, scale=-1.0),
                                  [ck], ["a1e"])
                            P.add("dve", lambda e, kb=kb: e.tensor_mul(out=kt_sb[kb][:], in0=k_sb[:], in1=e_sb[:]),
                                  ["a1k", "a1e"], ["a1kt%d" % kb])
                            P.add("act", lambda e, npz=npz: e.activation(out=e2_sb[:], in_=npz[:, :], func=AF.Exp),
                                  [nk_], ["a1e2"])
                            P.add("dve", lambda e, ob=ob: e.tensor_mul(out=kh_sb[ob][:], in0=k_sb[:], in1=e2_sb[:]),
                                  ["a1k", "a1e2"], ["a1kh%d" % ob])
                            P.dma("pool", khD[i, :, :], kh_sb[ob][:], ["a1kh%d" % ob], ["khD"], "a1kh%d" % ob)
                            P.add("dve", lambda e, ctp=ctp: e.tensor_copy(out=ct_sb[:], in_=ctp[:, :]), [ctk], ["a1ct"])
                            P.add("act", lambda e: e.activation(out=ecT[:], in_=ct_sb[:], func=AF.Exp),
                                  ["a1ct"], ["a1ecT"])
                            for c2 in range(2):
                                P.add("pool", lambda e, i=i, c2=c2: e.tensor_copy(
                                    out=totT[:, :, 2 * i + c2],
                                    in_=ct_sb[:].rearrange("p (h t) -> p h t", h=8)[:, :, c2 * 64 + 63]),
                                    ["a1ct"], ["totT"])
                            P.add("dve", lambda e, ob=ob: e.scalar_tensor_tensor(
                                out=qt_sb[ob][:], in0=sq_q[:], scalar=SCALE, in1=ecT[:], op0=ALU.mult, op1=ALU.mult),
                                ["a1sqq", "a1ecT"], ["a1qt%d" % ob])
                            P.dma("pool", qtT[i, :, :], qt_sb[ob][:], ["a1qt%d" % ob], ["qtT"], "a1qt%d" % ob)

                            def do_tp(i=i, ob=ob, kb=kb):
                                tp, tk = nps()
                                for h in range(8):
                                    P.add("pe", lambda e, h=h, tp=tp: e.transpose(
                                        out=tp[:, h * 128:(h + 1) * 128], in_=kt_sb[kb][:, h * 128:(h + 1) * 128],
                                        identity=ident[:]), ["a1kt%d" % kb, C], [tk])
                                P.add("dve", lambda e, tp=tp: e.tensor_copy(out=ktT_sb[ob][:], in_=tp[:, :]),
                                      [tk], ["a1ktT%d" % ob])
                                P.dma("pool", ktT[i, :, :], ktT_sb[ob][:], ["a1ktT%d" % ob], ["ktT"], "a1ktT%d" % ob)
                            pending_tp[0] = do_tp
                    if pending_tp[0] is not None:
                        pending_tp[0]()
                        pending_tp[0] = None
                P.barrier()
                chk("a1_%d" % l)

                with ExitStack() as st:
                    wB = sb("a2_w", [128, 8, 5120], BF16, st=st)
                    load_w_bf16(wB, w_in[l, :, 4096:9216], "a2w", 8, 5120)
                    xt = [sb("a2_xt%d" % i, [128, 8, 256], st=st) for i in range(2)]
                    sq2 = [sb("a2_sq%d" % i, [128, 8, 256], st=st) for i in range(2)]
                    rstd2 = [sb("a2_rstd%d" % i, [128, 256], st=st) for i in range(2)]
                    hT2 = [sb("a2_hT%d" % i, [128, 8, 256], BF16, st=st) for i in range(2)]
                    b_sb = sb("a2_b", [128, 8, 256], st=st)
                    vc_sb = [sb("a2_vc%d" % i, [128, 8, 256], st=st) for i in range(2)]
                    c_sb = [sb("a2_c%d" % i, [128, 8 * 256], BF16, st=st) for i in range(2)]
                    ga_sb = [sb("a2_ga%d" % i, [128, 8 * 256], BF16, st=st) for i in range(2)]
                    gb_sb = [sb("a2_gb%d" % i, [128, 8 * 256], BF16, st=st) for i in range(2)]

                    def ld_x2(j):
                        b = j % 2
                        P.dma("sp", xt[b][:], xT[:, :, j * 256:(j + 1) * 256].rearrange("c p t -> p c t"),
                              ["xT"], ["a2xt%d" % b], "a2xt%d" % b)
                    ld_x2(0)
                    if NT256 > 1:
                        ld_x2(1)

                    def norm2(j):
                        hb = j % 2
                        emit_norm(xt[hb][:], "a2xt%d" % hb, lambda dc: nmix[:, l, dc:dc + 1], hT2[hb][:], "a2hT%d" % hb,
                                  sq2[hb][:], "a2sq%d" % hb, rstd2[hb][:], "a2rstd%d" % hb, n=256)
                    norm2(0)

                    def proj2(col0, j):
                        res = []
                        for hf in range(2):
                            ps, pk = nps()
                            for c4 in range(4):
                                cc = hf * 4 + c4
                                for kc in range(8):
                                    P.add("pe", lambda e, ps=ps, c4=c4, cc=cc, kc=kc: e.matmul(
                                        ps[:, c4 * 256:(c4 + 1) * 256],
                                        lhsT=wB[:, kc, col0 + cc * 128:col0 + (cc + 1) * 128],
                                        rhs=hT2[j % 2][:, kc, :], start=(kc == 0), stop=(kc == 7)),
                                        ["a2hT%d" % (j % 2), "a2w"], [pk])
                            res.append((ps, pk))
                        return res

                    for j in range(NT256):
                        b = j % 2
                        if j + 1 < NT256:
                            norm2(j + 1)
                        if j + 2 < NT256:
                            ld_x2(j + 2)
                        for hf, (ps, pk) in enumerate(proj2(0, j)):
                            P.add("act", lambda e, ps=ps, hf=hf: e.copy(
                                out=b_sb[:, hf * 4:(hf + 1) * 4, :].rearrange("p c t -> p (c t)"), in_=ps[:, :]),
                                [pk], ["a2b"])
                        for hf, (ps, pk) in enumerate(proj2(1024, j)):
                            P.add("act", lambda e, ps=ps, hf=hf, b=b: e.copy(
                                out=c_sb[b][:, hf * 1024:(hf + 1) * 1024], in_=ps[:, :]), [pk], ["a2c%d" % b])
                        P.dma("pool", ccT[j, :, :], c_sb[b][:], ["a2c%d" % b], ["ccT"], "a2c%d" % b)
                        for hf, (ps, pk) in enumerate(proj2(2048, j)):
                            P.add("dve", lambda e, ps=ps, hf=hf, b=b: e.tensor_mul(
                                out=vc_sb[b][:, hf * 4:(hf + 1) * 4, :].rearrange("p c t -> p (c t)"),
                                in0=b_sb[:, hf * 4:(hf + 1) * 4, :].rearrange("p c t -> p (c t)"), in1=ps[:, :]),
                                [pk, "a2b"], ["a2vc%d" % b])
                        P.dma("pool", vcT[:, :, 2 + j * 256:2 + (j + 1) * 256].rearrange("c p t -> p c t"), vc_sb[b][:],
                              ["a2vc%d" % b], ["vcT"], "a2vc%d" % b)
                        for hf, (ps, pk) in enumerate(proj2(3072, j)):
                            P.add("act", lambda e, ps=ps, hf=hf, b=b: e.activation(
                                out=ga_sb[b][:, hf * 1024:(hf + 1) * 1024], in_=ps[:, :], func=AF.Sigmoid),
                                [pk], ["a2ga%d" % b])
                        P.dma("pool", gaT[j, :, :], ga_sb[b][:], ["a2ga%d" % b], ["gaT"], "a2ga%d" % b)
                        for hf, (ps, pk) in enumerate(proj2(4096, j)):
                            P.add("act", lambda e, ps=ps, hf=hf, b=b: e.activation(
                                out=gb_sb[b][:, hf * 1024:(hf + 1) * 1024], in_=ps[:, :], func=AF.Sigmoid),
                                [pk], ["a2gb%d" % b])
                        P.dma("pool", gbT[j, :, :], gb_sb[b][:], ["a2gb%d" % b], ["gbT"], "a2gb%d" % b)
                P.barrier()
                chk("a2_%d" % l)

                with ExitStack() as st:
                    S32 = sb("b_S32", [128, 8, 128], st=st)
                    SR = 4
                    Sbf = [sb("b_Sbf%d" % i, [128, 8, 128], BF16, st=st) for i in range(SR)]
                    Gp = sb("b_G", [128, 8], st=st)
                    EG = sb("b_EG", [128, 8], st=st)
                    dk = sb("b_dk", [128, 8, NCH], st=st)
                    qt = [sb("b_qt%d" % i, [128, 8, 128], BF16, st=st) for i in range(2)]
                    kt = [sb("b_kt%d" % i, [128, 8, 128], BF16, st=st) for i in range(2)]
                    kh = [sb("b_kh%d" % i, [128, 1024], BF16, st=st) for i in range(2)]
                    vt = [sb("b_vt%d" % i, [128, 1024], BF16, st=st) for i in range(2)]
                    at_sb = [sb("b_at%d" % i, [128, 8, 64], BF16, st=st) for i in range(2)]
                    o_sb = [sb("b_o%d" % i, [128, 1024], st=st) for i in range(2)]
                    qb_sb = [sb("b_qb%d" % i, [128, 8, 128], BF16, st=st) for i in range(2)]
                    P.add("dve", lambda e: e.memset(S32[:], 0.0), [], ["bS32_%d" % hh for hh in range(8)])
                    P.add("dve", lambda e: e.memset(Sbf[0][:], 0.0), [], ["bSbf0"])
                    P.add("dve", lambda e: e.memset(Gp[:], 0.0), [], ["bG"])
                    P.add("dve", lambda e: e.memset(EG[:], 1.0), [], ["bEG"])
                    P.add("act", lambda e: e.activation(out=dk[:], in_=totT[:], func=AF.Exp), ["totT"], ["bdk"])

                    def ld_b(i):
                        b = i % 2
                        P.dma("sp", qt[b][:].rearrange("p h t -> p (h t)"), qtT[i, :, :], ["qtT"], ["bqt%d" % b], "bqt%d" % b)
                        P.dma("sp", kt[b][:].rearrange("p h t -> p (h t)"), ktT[i, :, :], ["ktT"], ["bkt%d" % b], "bkt%d" % b)
                        P.dma("sp", kh[b][:], khD[i, :, :], ["khD"], ["bkh%d" % b], "bkh%d" % b)
                        P.dma("sp", vt[b][:], vtD[i, :, :], ["vtD"], ["bvt%d" % b], "bvt%d" % b)
                    ld_b(0)
                    for i in range(NT128):
                        b = i % 2
                        if i + 1 < NT128:
                            ld_b(i + 1)
                        ab2 = i % 2
                        ap_, ak = nps()
                        for c in range(2):
                            for h in range(8):
                                P.add("pe", lambda e, c=c, h=h, ap_=ap_, b=b: e.matmul(
                                    ap_[c * 64:(c + 1) * 64, h * 64:(h + 1) * 64],
                                    lhsT=kt[b][:, h, c * 64:(c + 1) * 64], rhs=qt[b][:, h, c * 64:(c + 1) * 64],
                                    start=True, stop=True), ["bkt%d" % b, "bqt%d" % b], [ak])
                        P.add("dve", lambda e, ap_=ap_, ab2=ab2: e.tensor_mul(
                            out=at_sb[ab2][:].rearrange("p h t -> p (h t)"), in0=ap_[:, 0:512],
                            in1=amask[:].rearrange("p h t -> p (h t)")), [ak, C], ["bat%d" % ab2])
                        ups = []
                        for c in range(2):
                            up, uk = nps()
                            for h in range(8):
                                P.add("pe", lambda e, c=c, h=h, up=up, b=b: e.matmul(
                                    up[:, h * 128:(h + 1) * 128],
                                    lhsT=kh[b][c * 64:(c + 1) * 64, h * 128:(h + 1) * 128],
                                    rhs=vt[b][c * 64:(c + 1) * 64, h * 128:(h + 1) * 128], start=True, stop=True),
                                    ["bkh%d" % b, "bvt%d" % b], [uk])
                            ups.append((up, uk))
                        op_, ok_ = nps()
                        for c in range(2):
                            ch = 2 * i + c
                            r0, r1 = ch % SR, (ch + 1) % SR
                            P.add("dve", lambda e, c=c, b=b: e.tensor_tensor(
                                out=qb_sb[b][:, :, c * 64:(c + 1) * 64], in0=qt[b][:, :, c * 64:(c + 1) * 64],
                                in1=EG[:].unsqueeze(2).broadcast_to([128, 8, 64]), op=ALU.mult),
                                ["bqt%d" % b, "bEG"], ["bqb%d_%d" % (b, c)])
                            P.add("dve", lambda e, ch=ch: e.tensor_add(out=Gp[:], in0=Gp[:], in1=totT[:, :, ch]),
                                  ["bG", "totT"], ["bG"])
                            P.add("act", lambda e: e.activation(out=EG[:], in_=Gp[:], func=AF.Exp), ["bG"], ["bEG"])
                            for h in range(8):
                                P.add("pe", lambda e, c=c, h=h, op_=op_, b=b, ab2=ab2: e.matmul(
                                    op_[:, h * 128 + c * 64:h * 128 + (c + 1) * 64],
                                    lhsT=vt[b][c * 64:(c + 1) * 64, h * 128:(h + 1) * 128],
                                    rhs=at_sb[ab2][c * 64:(c + 1) * 64, h, :], start=True, stop=False),
                                    ["bvt%d" % b, "bat%d" % ab2], [ok_])
                                P.add("pe", lambda e, c=c, h=h, op_=op_, b=b, r0=r0: e.matmul(
                                    op_[:, h * 128 + c * 64:h * 128 + (c + 1) * 64],
                                    lhsT=Sbf[r0][:, h, :], rhs=qt[b][:, h, c * 64:(c + 1) * 64], start=False, stop=True),
                                    ["bSbf%d" % r0, "bqt%d" % b], [ok_])
                            up, uk = ups[c]
                            for h in range(8):
                                P.add("dve", lambda e, h=h, up=up, ch=ch: e.scalar_tensor_tensor(
                                    out=S32[:, h, :], in0=S32[:, h, :], scalar=dk[:, h, ch:ch + 1],
                                    in1=up[:, h * 128:(h + 1) * 128], op0=ALU.mult, op1=ALU.add),
                                    ["bS32_%d" % h, "bdk", uk], ["bS32_%d" % h])
                            P.add("act", lambda e, r1=r1: e.copy(out=Sbf[r1][:], in_=S32[:]),
                                  ["bS32_%d" % hh for hh in range(8)], ["bSbf%d" % r1])
                        P.add("act", lambda e, op_=op_, b=b: e.copy(out=o_sb[b][:], in_=op_[:, :]), [ok_], ["bo%d" % b])
                        P.dma("pool", opT[i, :, :], o_sb[b][:], ["bo%d" % b], ["opT"], "bo%d" % b)
                        P.dma("pool", qbT[i, :, :], qb_sb[b][:].rearrange("p h t -> p (h t)"), ["bqb%d_%d" % (b, hh) for hh in range(2)], ["qbT"], "bqb%d" % b)
                    P.dma("pool", gin[0:1024, :].rearrange("(h k) v -> k h v", h=8), S32[:], ["bS32_%d" % hh for hh in range(8)], ["gin"], "bgin")
                    P.dma("pool", gin[1024:1040, :].rearrange("(c r) (q t) -> c (r q) t", c=8, t=2),
                          vcT[:, :, NT:NT + 2], ["vcT"], ["gin"], "bgin")
                P.barrier()
                chk("b_%d" % l)
                P.add("pool", lambda e: e.collective_compute(
                    "AllGather", ALU.bypass, replica_groups=[[2 * i, 2 * i + 1] for i in range(NCORES // 2)],
                    ins=[gin[:, :]], outs=[gout[:, :]]), ["gin"], ["gout"], ainc=1, semkey="cc%d" % l)
                P.barrier()
                chk("cc_%d" % l)

                with ExitStack() as st:
                    wph = sb("c_wph", [128, 8, 1024], BF16, st=st)
                    wpc = sb("c_wpc", [128, 8, 1024], BF16, st=st)
                    wo = sb("c_wo", [128, 8, 1024], BF16, st=st)
                    load_w_bf16(wph, wph_in[l], "cw", 8, 1024)
                    load_w_bf16(wpc, wpc_in[l], "cw", 8, 1024)
                    load_w_bf16(wo, wo_in[l], "cw", 8, 1024)
                    SA32 = sb("c_SA32", [128, 8, 128], st=st)
                    SAbf = sb("c_SAbf", [128, 8, 128], BF16, st=st)
                    halo = sb("c_halo", [128, 8, 2], st=st)
                    P.dma("sp", SA32[:], gout[0:1024, :].rearrange("(h k) v -> k h v", h=8), ["gout"], ["cSA32"], "cSA")
                    P.dma("sp", halo[:], gout[1024:1040, :].rearrange("(c r) (q t) -> (r q) c t", c=8, t=2),
                          ["gout"], ["chalo"], "cSA")
                    P.add("dve", lambda e: e.tensor_scalar(out=SAbf[:], in0=SA32[:], scalar1=sel[:, 0:1], scalar2=None,
                                                           op0=ALU.mult), ["cSA32", C], ["cSAbf"])
                    P.add("dve", lambda e: e.tensor_scalar(out=halo[:], in0=halo[:], scalar1=sel[:, 0:1], scalar2=None,
                                                           op0=ALU.mult), ["chalo", C], ["chalo"])
                    P.dma("sp", vcT[:, :, 0:2].rearrange("c p t -> p c t"), halo[:], ["chalo"], ["vcT"], "chalo")
                    N = 128
                    N2 = 256
                    NB = 3
                    NP = NT // N2
                    op_sb = [sb("c_op%d" % i, [128, 1024], st=st) for i in range(NB)]
                    qb_sb = [sb("c_qb%d" % i, [128, 1024], BF16, st=st) for i in range(NB)]
                    gs_sb = [sb("c_gs%d" % i, [128, 1024], BF16, st=st) for i in range(NB)]
                    vc_sb = [sb("c_vc%d" % i, [128, 8, N + 2], st=st) for i in range(NB)]
                    cc_sb = [sb("c_cc%d" % i, [128, 8, N], BF16, st=st) for i in range(NB)]
                    ga_sb = [sb("c_ga%d" % i, [128, 8 * N2], BF16, st=st) for i in range(2)]
                    gb_sb = [sb("c_gb%d" % i, [128, 8 * N2], BF16, st=st) for i in range(2)]
                    xt_sb = [sb("c_xt%d" % i, [128, 8, N2], st=st) for i in range(2)]
                    sqo2 = [sb("c_sqo%d" % i, [128, 1024], st=st) for i in range(2)]
                    rstd2 = [sb("c_rstd%d" % i, [128, 1024], st=st) for i in range(2)]
                    on_bf = [sb("c_on%d" % i, [128, 8, N2], BF16, st=st) for i in range(2)]
                    cdiag = sb("c_cdiag", [128, 24, 128], st=st)
                    for jc in range(24):
                        P.add("dve", lambda e, jc=jc: e.tensor_scalar(
                            out=cdiag[:, jc, :], in0=ident[:], scalar1=convw[:, l, jc:jc + 1], scalar2=None,
                            op0=ALU.mult), [C], ["cdiag"])
                    yc_bf = [sb("c_yc%d" % i, [128, 8, N2], BF16, st=st) for i in range(2)]
                    m1 = sb("c_m1", [128, 8 * N2], st=st)
                    m2 = sb("c_m2", [128, 8 * N2], st=st)
                    mg_bf = [sb("c_mg%d" % i, [128, 8, N2], BF16, st=st) for i in range(2)]

                    def ld_t(i):
                        b = i % NB
                        j2, s2 = i // 2, i % 2
                        P.dma("sp", op_sb[b][:], opT[i, :, :], ["opT"], ["cop%d" % b], "cld%d" % b)
                        P.dma("sp", qb_sb[b][:], qbT[i, :, :], ["qbT"], ["cqb%d" % b], "cld%d" % b)
                        P.dma("sp", gs_sb[b][:], gsT[i, :, :], ["gsT"], ["cgs%d" % b], "cld%d" % b)
                        P.dma("sp", vc_sb[b][:], vcT[:, :, i * N:(i + 1) * N + 2].rearrange("c p t -> p c t"),
                              ["vcT"], ["cvc%d" % b], "cld%d" % b)
                        P.dma("sp", cc_sb[b][:],
                              ccT[j2, :, :].rearrange("p (c t) -> p c t", c=8)[:, :, s2 * 128:(s2 + 1) * 128],
                              ["ccT"], ["ccc%d" % b], "cld%d" % b)

                    def ld_g(p):
                        b = p % 2
                        P.dma("sp", ga_sb[b][:], gaT[p, :, :], ["gaT"], ["cga%d" % b], "clg%d" % b)
                        P.dma("sp", gb_sb[b][:], gbT[p, :, :], ["gbT"], ["cgb%d" % b], "clg%d" % b)

                    def ld_x(p):
                        b = p % 2
                        P.dma("sp", xt_sb[b][:], xT[:, :, p * N2:(p + 1) * N2].rearrange("c p t -> p c t"),
                              ["xTc%d" % p], ["cxt%d" % b], "clx%d" % b)

                    def mmpair(w, rhs, rkeys, wk):
                        res = []
                        for hf in range(2):
                            ps, pk = nps()
                            for d4 in range(4):
                                dc = hf * 4 + d4
                                for kc in range(8):
                                    P.add("pe", lambda e, ps=ps, d4=d4, dc=dc, kc=kc: e.matmul(
                                        ps[:, d4 * N2:(d4 + 1) * N2], lhsT=w[:, kc, dc * 128:(dc + 1) * 128],
                                        rhs=rhs[:, kc, :], start=(kc == 0), stop=(kc == 7)), list(rkeys) + [wk], [pk])
                            res.append((ps, pk))
                        return res

                    def stage_h1(i):
                        b = i % NB
                        pp = (i // 2) % 2
                        hs = i % 2
                        csl = slice(hs * N, (hs + 1) * N)
                        sqo, rstd = sqo2[hs], rstd2[hs]
                        sqk, rsk = "csqo%d" % hs, "crstd%d" % hs
                        ps, pk = nps()
                        for h in range(8):
                            P.add("pe", lambda e, ps=ps, h=h, b=b: e.matmul(
                                ps[:, h * N:(h + 1) * N], lhsT=SAbf[:, h, :], rhs=qb_sb[b][:, h * N:(h + 1) * N],
                                start=True, stop=True), ["cSAbf", "cqb%d" % b], [pk])
                        P.add("dve", lambda e, ps=ps, b=b: e.tensor_add(out=op_sb[b][:], in0=op_sb[b][:], in1=ps[:, :]),
                              [pk, "cop%d" % b], ["cop%d" % b])
                        P.add("act", lambda e, b=b: e.activation(out=sqo[:], in_=op_sb[b][:], func=AF.Square),
                              ["cop%d" % b], [sqk])
                        ps, pk = nps()
                        for h in range(8):
                            P.add("pe", lambda e, ps=ps, h=h: e.matmul(
                                ps[:, h * N:(h + 1) * N], lhsT=ones[:], rhs=sqo[:, h * N:(h + 1) * N],
                                start=True, stop=True), [sqk, "ones"], [pk])
                        P.add("act", lambda e, ps=ps: e.activation(out=rstd[:], in_=ps[:, :], func=AF.Ln,
                                                                   scale=1.0 / 128, bias=epsb[:]), [pk, "epsb"], [rsk])
                        P.add("act", lambda e: e.activation(out=rstd[:], in_=rstd[:], func=AF.Exp, scale=-0.5),
                              [rsk], [rsk])
                        P.add("dve", lambda e, b=b: e.scalar_tensor_tensor(
                            out=sqo[:], in0=op_sb[b][:], scalar=gnw[:, l:l + 1], in1=rstd[:],
                            op0=ALU.mult, op1=ALU.mult), ["cop%d" % b, rsk, C, sqk], [sqk])
                        P.add("dve", lambda e, b=b, pp=pp, csl=csl: e.tensor_mul(
                            out=on_bf[pp][:, :, csl], in0=sqo[:].rearrange("p (h t) -> p h t", h=8),
                            in1=gs_sb[b][:].rearrange("p (h t) -> p h t", h=8)),
                            [sqk, "cgs%d" % b], ["con%d_%d" % (pp, hs)])
                        cps, cpk = nps()
                        for cc in range(8):
                            for j in range(3):
                                P.add("pe", lambda e, cps=cps, cc=cc, j=j, b=b: e.matmul(
                                    cps[:, cc * N:(cc + 1) * N], lhsT=cdiag[:, j * 8 + cc, :],
                                    rhs=vc_sb[b][:, cc, j:N + j], start=(j == 0), stop=(j == 2)),
                                    ["cvc%d" % b, "cdiag"], [cpk])
                        P.add("dve", lambda e, cps=cps, b=b, pp=pp, csl=csl: e.tensor_mul(
                            out=yc_bf[pp][:, :, csl], in0=cps[:, :].rearrange("p (c t) -> p c t", c=8), in1=cc_sb[b][:]),
                            [cpk, "ccc%d" % b], ["cyc%d_%d" % (pp, hs)])

                    def stage_h2a(p):
                        b = p % 2
                        ya = mmpair(wph, on_bf[b], ["con%d_0" % b, "con%d_1" % b], "cw")
                        for hf in range(2):
                            pa, pak = ya[hf]
                            P.add("dve", lambda e, pa=pa, b=b, hf=hf: e.tensor_mul(
                                out=m1[:, hf * 4 * N2:(hf + 1) * 4 * N2], in0=pa[:, :],
                                in1=ga_sb[b][:, hf * 4 * N2:(hf + 1) * 4 * N2]), [pak, "cga%d" % b], ["cm1_%d" % hf])
                        yb = mmpair(wpc, yc_bf[b], ["cyc%d_0" % b, "cyc%d_1" % b], "cw")
                        for hf in range(2):
                            pb, pbk = yb[hf]
                            P.add("dve", lambda e, pb=pb, b=b, hf=hf: e.tensor_mul(
                                out=m2[:, hf * 4 * N2:(hf + 1) * 4 * N2], in0=pb[:, :],
                                in1=gb_sb[b][:, hf * 4 * N2:(hf + 1) * 4 * N2]), [pbk, "cgb%d" % b], ["cm2_%d" % hf])
                        P.add("dve", lambda e, b=b: e.tensor_add(
                            out=mg_bf[b][:].rearrange("p c t -> p (c t)"), in0=m1[:], in1=m2[:]),
                            ["cm1_0", "cm1_1", "cm2_0", "cm2_1"], ["cmg%d" % b])

                    def stage_h2b(p):
                        b = p % 2
                        yo = mmpair(wo, mg_bf[b], ["cmg%d" % b], "cw")
                        for hf in range(2):
                            po, pok = yo[hf]
                            P.add("dve", lambda e, po=po, b=b, hf=hf: e.tensor_add(
                                out=xt_sb[b][:, hf * 4:(hf + 1) * 4, :].rearrange("p c t -> p (c t)"),
                                in0=xt_sb[b][:, hf * 4:(hf + 1) * 4, :].rearrange("p c t -> p (c t)"), in1=po[:, :]),
                                [pok, "cxt%d" % b], ["cxt%d" % b])
                        P.dma("pool", xT[:, :, p * N2:(p + 1) * N2].rearrange("c p t -> p c t"), xt_sb[b][:],
                              ["cxt%d" % b], ["xTc%d" % p], "cxts%d" % b)

                    ld_t(0)
                    ld_t(1)
                    ld_g(0)
                    ld_x(0)
                    if NP > 1:
                        ld_x(1)
                    for p in range(NP + 2):
                        if p < NP:
                            stage_h1(2 * p)
                            stage_h1(2 * p + 1)
                            if 2 * p + 2 < NT128:
                                ld_t(2 * p + 2)
                                ld_t(2 * p + 3)
                        if 0 <= p - 1 < NP:
                            stage_h2a(p - 1)
                        if 0 <= p - 2 < NP:
                            stage_h2b(p - 2)
                        if p + 1 < NP:
                            ld_g(p + 1)
                        if p >= 2 and p < NP:
                            ld_x(p)
                P.barrier()
                chk("c_%d" % l)
                if dbg and ("xT_mix%d" % l) in dbg_outs:
                    P.dma("sp", dbg_outs["xT_mix%d" % l].rearrange("(c p) t -> c p t", p=128), xT[:, :, :], ["xT"], [], "dbg")
                    P.barrier()

                moe = (l % 2 == 1)
                nexp = NE if moe else 1
                WB = eblob_in if moe else dblob_in
                with ExitStack() as st:
                    acc = sb("f_acc", [128, 8, HALF], st=st)
                    hT = sb("f_hT", [128, 8, HALF], BF16, st=st)
                    sq = sb("f_sq", [128, 8, 512], st=st)
                    rstd = sb("f_rstd", [128, 512], st=st)
                    wt = [sb("f_wt%d" % i, [128, 6144], BF16, st=st) for i in range(2)]
                    sa = [sb("f_sa%d" % i, [128, 512], st=st) for i in range(2)]
                    pr = [sb("f_pr%d" % i, [128, 512], st=st) for i in range(2)]
                    act = [sb("f_act%d" % i, [128, 512], BF16, st=st) for i in range(4)]
                    if moe:
                        gbc = sb("f_gbc", [128, NE, HALF], BF16, st=st)
                        lg = sb("f_lg", [128, 4, NE], st=st)
                        l2 = sb("f_l2", [128, 4, NE], st=st)
                        mx = sb("f_mx", [128, 2, 4], st=st)
                        msk = sb("f_msk", [128, 4, NE], st=st)
                        gt = sb("f_gt", [128, 4, NE], st=st)
                        den = sb("f_den", [128, 4], st=st)
                        gexp2 = [sb("f_gexp%d" % i, [128, NE, 128], st=st) for i in range(2)]
                    for hv in range(NHALF):
                        t0 = hv * HALF
                        for tt in range(HALF // 512):
                            P.dma("sp", acc[:, :, tt * 512:(tt + 1) * 512],
                                  xT[:, :, t0 + tt * 512:t0 + (tt + 1) * 512].rearrange("c p t -> p c t"),
                                  ["xT"], ["facc%d" % tt], "facc%d" % tt)
                        for tt in range(HALF // 512):
                            tsl = slice(tt * 512, (tt + 1) * 512)
                            if moe:
                                emit_norm(acc[:, :, tsl], "facc%d" % tt, lambda dc: nffn[:, l, dc:dc + 1], hT[:, :, tsl], "fhT",
                                          sq[:], "fsq", rstd[:], "frstd", h32=sq[:], h32k="fsq")
                                ps, pk = nps()
                                for s in range(4):
                                    for kc in range(8):
                                        P.add("pe", lambda e, ps=ps, kc=kc, s=s: e.matmul(
                                            ps[:, s * NE:(s + 1) * NE], lhsT=sq[:, kc, s * 128:(s + 1) * 128], rhs=rw[:, kc, :],
                                            start=(kc == 0), stop=(kc == 7)), ["fsq", C], [pk])
                                f2 = lambda t: t[:].rearrange("p s e -> p (s e)")
                                bc = lambda v: v.unsqueeze(2).broadcast_to([128, 4, NE])
                                P.add("dve", lambda e, ps=ps: e.tensor_copy(out=f2(lg), in_=ps[:, 0:4 * NE]), [pk], ["flg"])
                                P.add("dve", lambda e: e.tensor_reduce(out=mx[:, 0, :], in_=lg[:], axis=AX.X, op=ALU.max),
                                      ["flg"], ["fmx"])
                                P.add("dve", lambda e: e.tensor_tensor(out=msk[:], in0=lg[:], in1=bc(mx[:, 0, :]), op=ALU.is_ge),
                                      ["flg", "fmx"], ["fmsk"])
                                P.add("dve", lambda e: e.scalar_tensor_tensor(out=f2(l2), in0=f2(msk), scalar=-1e30, in1=f2(lg),
                                                                              op0=ALU.mult, op1=ALU.add), ["fmsk", "flg"], ["fl2"])
                                P.add("dve", lambda e: e.tensor_reduce(out=mx[:, 1, :], in_=l2[:], axis=AX.X, op=ALU.max),
                                      ["fl2"], ["fmx"])
                                P.add("dve", lambda e: e.tensor_tensor(out=msk[:], in0=lg[:], in1=bc(mx[:, 1, :]), op=ALU.is_ge),
                                      ["flg", "fmx"], ["fmsk"])
                                P.add("dve", lambda e: e.tensor_tensor(out=gt[:], in0=lg[:], in1=bc(mx[:, 0, :]), op=ALU.subtract),
                                      ["flg", "fmx"], ["fgt"])
                                P.add("act", lambda e: e.activation(out=f2(gt), in_=f2(gt), func=AF.Exp), ["fgt"], ["fgt"])
                                P.add("dve", lambda e: e.tensor_mul(out=f2(gt), in0=f2(gt), in1=f2(msk)), ["fgt", "fmsk"], ["fgt"])
                                P.add("dve", lambda e: e.tensor_reduce(out=den[:], in_=gt[:], axis=AX.X, op=ALU.add),
                                      ["fgt"], ["fden"])
                                P.add("dve", lambda e: e.reciprocal(out=den[:], in_=den[:]), ["fden"], ["fden"])
                                P.add("dve", lambda e: e.tensor_tensor(out=gt[:], in0=gt[:], in1=bc(den[:]), op=ALU.mult),
                                      ["fgt", "fden"], ["fgt"])
                                for s in range(4):
                                    gx = gexp2[s % 2]
                                    gk = "fgexp%d" % (s % 2)
                                    P.add("dve", lambda e, s=s, gx=gx: e.tensor_copy(
                                        out=gx[:], in_=gt[:, s, :].unsqueeze(2).broadcast_to([128, NE, 128])),
                                        ["fgt"], [gk])
                                    ps, pk = nps()
                                    for ee in range(NE):
                                        P.add("pe", lambda e, ps=ps, ee=ee, gx=gx: e.matmul(
                                            ps[:, ee * 128:(ee + 1) * 128], lhsT=gx[:, ee, :], rhs=ident[:],
                                            start=True, stop=True), [gk, C], [pk])
                                    P.add("act", lambda e, ps=ps, tt=tt, s=s: e.copy(
                                        out=gbc[:, :, tt * 512 + s * 128:tt * 512 + (s + 1) * 128],
                                        in_=ps[:, :].rearrange("p (e t) -> p e t", e=NE)), [pk], ["fgbc"])
                            else:
                                emit_norm(acc[:, :, tsl], "facc%d" % tt, lambda dc: nffn[:, l, dc:dc + 1], hT[:, :, tsl], "fhT",
                                          sq[:], "fsq", rstd[:], "frstd")
                        NFG = DFF // 256
                        NTT = HALF // 512
                        groups = [(ex, fg) for ex in range(nexp) for fg in range(NFG)]
                        PA = [(PS[0], "ps0"), (PS[1], "ps1")]
                        PY = [(PS[2], "ps2"), (PS[3], "ps3")]

                        def ld_w(gi):
                            ex, fg = groups[gi]
                            wb = gi % 2
                            P.dma("pool", wt[wb][:], WB[ex, fg, :, :], [], ["fw%d" % wb], "fw%d" % wb)

                        def emit_ab(gi, tt, n):
                            ex, fg = groups[gi]
                            wb = gi % 2
                            tsl = slice(tt * 512, (tt + 1) * 512)
                            for fc in range(2):
                                pa, pak = PA[fc]
                                for kc in range(8):
                                    P.add("pe", lambda e, pa=pa, kc=kc, fc=fc, wb=wb, tsl=tsl: e.matmul(
                                        pa[:, 0:512], lhsT=wt[wb][:, kc * 256 + fc * 128:kc * 256 + (fc + 1) * 128],
                                        rhs=hT[:, kc, tsl], start=(kc == 0), stop=(kc == 7)), ["fhT", "fw%d" % wb], [pak])
                                for kc in range(8):
                                    P.add("pe", lambda e, pa=pa, kc=kc, fc=fc, wb=wb, tsl=tsl: e.matmul(
                                        pa[:, 512:1024], lhsT=wt[wb][:, 2048 + kc * 256 + fc * 128:2048 + kc * 256 + (fc + 1) * 128],
                                        rhs=hT[:, kc, tsl], start=(kc == 0), stop=(kc == 7)), ["fhT", "fw%d" % wb], [pak])
                                ai = 2 * (n % 2) + fc
                                P.add("act", lambda e, pa=pa, fc=fc: e.activation(out=sa[fc][:], in_=pa[:, 0:512], func=AF.Silu),
                                      [pak], ["fsa%d" % fc])
                                if moe:
                                    P.add("dve", lambda e, pa=pa, fc=fc: e.tensor_mul(out=pr[fc][:], in0=sa[fc][:], in1=pa[:, 512:1024]),
                                          [pak, "fsa%d" % fc], ["fpr%d" % fc])
                                    P.add("dve", lambda e, fc=fc, ai=ai, ex=ex, tsl=tsl: e.tensor_mul(
                                        out=act[ai][:], in0=pr[fc][:], in1=gbc[:, ex, tsl]), ["fpr%d" % fc, "fgbc"], ["fact%d" % ai])
                                else:
                                    P.add("dve", lambda e, pa=pa, fc=fc, ai=ai: e.tensor_mul(out=act[ai][:], in0=sa[fc][:], in1=pa[:, 512:1024]),
                                          [pak, "fsa%d" % fc], ["fact%d" % ai])

                        def emit_y(gi, tt, n):
                            wb = gi % 2
                            tsl = slice(tt * 512, (tt + 1) * 512)
                            for dh in range(4):
                                py, pyk = PY[dh % 2]
                                for d2 in range(2):
                                    dc = dh * 2 + d2
                                    for fc in range(2):
                                        ai = 2 * (n % 2) + fc
                                        P.add("pe", lambda e, py=py, d2=d2, dc=dc, fc=fc, wb=wb, ai=ai: e.matmul(
                                            py[:, d2 * 512:(d2 + 1) * 512],
                                            lhsT=wt[wb][:, 4096 + fc * 1024 + dc * 128:4096 + fc * 1024 + (dc + 1) * 128],
                                            rhs=act[ai][:], start=(fc == 0), stop=(fc == 1)),
                                            ["fw%d" % wb, "fact%d" % ai], [pyk])
                                P.add("dve", lambda e, py=py, dh=dh, tsl=tsl: e.tensor_add(
                                    out=acc[:, dh * 2:dh * 2 + 2, tsl], in0=acc[:, dh * 2:dh * 2 + 2, tsl],
                                    in1=py[:, :].rearrange("p (c t) -> p c t", c=2)), [pyk, "facc%d" % tt], ["facc%d" % tt])

                        ld_w(0)
                        if len(groups) > 1:
                            ld_w(1)
                        prev = None
                        n = 0
                        for gi in range(len(groups)):
                            for tt in range(NTT):
                                emit_ab(gi, tt, n)
                                if prev is not None:
                                    emit_y(*prev)
                                if tt == 0 and gi >= 1 and gi + 1 < len(groups):
                                    ld_w(gi + 1)
                                prev = (gi, tt, n)
                                n += 1
                        emit_y(*prev)
                        for tt in range(HALF // 512):
                            P.dma("pool", xT[:, :, t0 + tt * 512:t0 + (tt + 1) * 512].rearrange("c p t -> p c t"),
                                  acc[:, :, tt * 512:(tt + 1) * 512], ["facc%d" % tt], ["xT"], "faccst%d" % tt)
                P.barrier()
                chk("ffn_%d" % l)
                if dbg and ("xT_ffn%d" % l) in dbg_outs:
                    P.dma("sp", dbg_outs["xT_ffn%d" % l].rearrange("(c p) t -> c p t", p=128), xT[:, :, :], ["xT"], [], "dbg")
                    P.barrier()

            with ExitStack() as st:
                xt = [sb("z_xt%d" % i, [128, 8, 512], st=st) for i in range(2)]
                sq = sb("z_sq", [128, 8, 512], st=st)
                rstd = sb("z_rstd", [128, 512], st=st)
                hn2 = [sb("z_hn%d" % i, [128, 8, 512], st=st) for i in range(2)]
                yo = [sb("z_yo%d" % i, [128, 1024], st=st) for i in range(2)]

                def ld_z(j):
                    b = j % 2
                    P.dma("sp", xt[b][:], xT[:, :, j * 512:(j + 1) * 512].rearrange("c p t -> p c t"),
                          ["xT"], ["zxt%d" % b], "zxt%d" % b)
                ld_z(0)
                for j in range(NT512):
                    b = j % 2
                    if j + 1 < NT512:
                        ld_z(j + 1)
                    hn = hn2[b]
                    emit_norm(xt[b][:], "zxt%d" % b, lambda dc: fnw[:, dc:dc + 1], hn[:], "zhn%d" % b, sq[:], "zsq", rstd[:], "zrstd")
                    for s in range(4):
                        ob = (j * 4 + s) % 2
                        ps, pk = nps()
                        for dc in range(8):
                            P.add("pe", lambda e, ps=ps, dc=dc, s=s, hn=hn: e.transpose(
                                out=ps[:, dc * 128:(dc + 1) * 128], in_=hn[:, dc, s * 128:(s + 1) * 128], identity=ident[:]),
                                ["zhn%d" % b, C], [pk])
                        P.add("dve", lambda e, ps=ps, ob=ob: e.tensor_copy(out=yo[ob][:], in_=ps[:, :]), [pk], ["zyo%d" % ob])
                        r0 = j * 512 + s * 128
                        P.dma("pool", out_d[r0:r0 + 128, :], yo[ob][:], ["zyo%d" % ob], [], "zyo%d" % ob)
        except _Stop:
            P.barrier()
            P.dma("sp", dbg_outs["stop"].rearrange("(c p) t -> c p t", p=128), xT[:, :, :], ["xT"], [], "dbg")
        nsem = P.emit()
    return nc, nsem, len(P.ops)


def make_consts():
    ident = np.eye(128, dtype=np.float32)
    s = np.arange(128)[:, None]
    t = np.arange(128)[None, :]
    same = (s // 64) == (t // 64)
    triu = (same & (s <= t)).astype(np.float32)
    tris = (same & (s > t)).astype(np.float32)
    sl = (np.arange(128) % 64)[:, None]
    tt = np.arange(64)[None, :]
    m = (sl <= tt).astype(np.float32)
    mask = np.tile(m[:, None, :], (1, 8, 1)).reshape(128, 512)
    return ident, triu, tris, np.ascontiguousarray(mask)


def ffn_blob(w1, w3, w2):
    w1 = np.asarray(w1, dtype=np.float32)
    w3 = np.asarray(w3, dtype=np.float32)
    w2 = np.asarray(w2, dtype=np.float32)
    ne = w1.shape[0]
    nfg = DFF // 256
    a = w1.reshape(ne, 8, 128, nfg, 256).transpose(0, 3, 2, 1, 4).reshape(ne, nfg, 128, 2048)
    b = w3.reshape(ne, 8, 128, nfg, 256).transpose(0, 3, 2, 1, 4).reshape(ne, nfg, 128, 2048)
    c = w2.reshape(ne, nfg, 2, 128, D).transpose(0, 1, 3, 2, 4).reshape(ne, nfg, 128, 2048)
    return np.ascontiguousarray(np.concatenate([a, b, c], axis=-1))


def layout_inputs(inp, seq, nt):
    f = lambda a: np.ascontiguousarray(np.asarray(a, dtype=np.float32))
    ident, triu, tris, mask = make_consts()
    shared = {
        "w_in": f(inp["w_in"]),
        "lower_bounds": f(inp["lower_bounds"]),
        "hgrn_norm_w": f(inp["hgrn_norm_w"]).reshape(DEPTH, 128, 1),
        "conv_w": f(np.asarray(inp["conv_w"]).reshape(DEPTH, 3, 8, 128).transpose(0, 3, 1, 2).reshape(DEPTH, 128, 24)),
        "w_proj_hgrn": f(inp["w_proj_hgrn"]),
        "w_proj_conv": f(inp["w_proj_conv"]),
        "w_out": f(inp["w_out"]),
        "norm_mix": f(np.asarray(inp["norm_mix"]).reshape(DEPTH, 8, 128).transpose(0, 2, 1)),
        "norm_ffn": f(np.asarray(inp["norm_ffn"]).reshape(DEPTH, 8, 128).transpose(0, 2, 1)),
        "dense_blob": ffn_blob(inp["dense_w1"], inp["dense_w3"], inp["dense_w2"]),
        "router_w": f(np.asarray(inp["router_w"]).reshape(8, 128, NE).transpose(1, 0, 2)),
        "expert_blob": ffn_blob(np.asarray(inp["expert_w1"])[0], np.asarray(inp["expert_w3"])[0],
                                np.asarray(inp["expert_w2"])[0]),
        "final_norm": f(np.asarray(inp["final_norm"]).reshape(8, 128).T),
        "c_ident": ident, "c_triu": triu, "c_tris": tris, "c_mask": mask,
    }
    x = np.asarray(inp["x"], dtype=np.float32)
    maps = []
    for c in range(NCORES):
        b, hf = c // 2, c % 2
        m = dict(shared)
        m["x"] = np.ascontiguousarray(x[b, hf * nt:(hf + 1) * nt, :])
        m["c_sel"] = np.full((128, 1), float(hf), dtype=np.float32)
        maps.append(m)
    return maps


_CACHE = {}


def run(inputs, dbg=None, trace=False):
    x = np.asarray(inputs["x"])
    bsz, seq, _ = x.shape
    assert bsz * 2 == NCORES
    nt = seq // 2
    key = (nt, tuple(sorted(dbg.items())) if dbg else None)
    if key not in _CACHE:
        _CACHE[key] = build_program(nt, dbg)[0]
    nc = _CACHE[key]
    maps = layout_inputs(inputs, seq, nt)
    res = run_bass_kernel_spmd(nc, maps, core_ids=list(range(NCORES)), **({"trace": True} if trace else {}))
    out = np.empty((bsz, seq, D), dtype=np.float32)
    for c in range(NCORES):
        b, hf = c // 2, c % 2
        out[b, hf * nt:(hf + 1) * nt, :] = res.results[c]["out"]
    return out, res


def kernel(**inputs):
    out, _ = run(inputs)
    return out
```

```python
import numpy as np
from contextlib import ExitStack
import concourse.bass as bass
import concourse.mybir as mybir
from concourse.bass_utils import run_bass_kernel_spmd

F32 = mybir.dt.float32
BF16 = mybir.dt.bfloat16
AF = mybir.ActivationFunctionType
ALU = mybir.AluOpType
AX = mybir.AxisListType

D = 1024
NH = 8
DFF = 2816
NE = 8
DEPTH = 2
INW = 9216
EPS = 1e-6
F_MIN = 1e-6
SCALE = 128 ** -0.5
NCORES = 8


class _Rec:
    def __init__(self):
        self.call = None

    def __getattr__(self, name):
        def f(*a, **k):
            self.call = (name, a, k)
        return f


class Prog:
    ENG = ("pe", "act", "dve", "pool", "sp")

    def __init__(self, nc):
        self.nc = nc
        self.ops = []
        self.last_w = {}
        self.readers = {}
        self.last_on = {}
        self.asyncs = []

    def add(self, eng, fn, reads=(), writes=(), ainc=0, semkey=None):
        i = len(self.ops)
        deps = set()
        for k in reads:
            if k in self.last_w:
                deps.add(self.last_w[k])
        for k in writes:
            if k in self.last_w:
                deps.add(self.last_w[k])
            deps.update(self.readers.get(k, ()))
        for k in reads:
            self.readers.setdefault(k, []).append(i)
        for k in writes:
            self.last_w[k] = i
            self.readers[k] = []
        if fn is not None:
            rec = _Rec()
            fn(rec)
            name_, a_, k_ = rec.call
            fn = (lambda e, name_=name_, a_=a_, k_=k_: getattr(e, name_)(*a_, **k_))
        self.ops.append(dict(eng=eng, fn=fn, deps=deps, ainc=ainc, semkey=semkey, signal=False))
        if ainc:
            self.asyncs.append(i)
        elif fn is not None:
            self.last_on[eng] = i
        return i

    def dma(self, eng, out, in_, reads, writes, semkey, **kw):
        return self.add(eng, lambda e: e.dma_start(out=out, in_=in_, **kw), reads, writes,
                        ainc=16, semkey=(eng, semkey))

    def barrier(self):
        deps = set(self.last_on.values()) | set(self.asyncs)
        for e in self.ENG:
            i = self.add(e, None)
            self.ops[i]["deps"] = set(deps)
        self.asyncs = []

    @staticmethod
    def _skip(od, o):
        return (not od["ainc"]) and od["eng"] == "pe" and o["eng"] == "pe" and not o["ainc"] \
            and o["fn"] is not None

    def emit(self, final_wait_eng="sp"):
        nc = self.nc
        ops = self.ops
        for o in ops:
            for d in o["deps"]:
                od = ops[d]
                if od["ainc"] or self._skip(od, o):
                    continue
                od["signal"] = True
        eng_cnt = {e: 0 for e in self.ENG}
        a_cnt = {}
        for o in ops:
            if o["ainc"]:
                a_cnt[o["semkey"]] = a_cnt.get(o["semkey"], 0) + o["ainc"]
                o["cnt"] = a_cnt[o["semkey"]]
            elif o["signal"]:
                eng_cnt[o["eng"]] += 1
                o["cnt"] = eng_cnt[o["eng"]]
        running = {}
        for o in ops:
            waits = {}
            for d in o["deps"]:
                od = ops[d]
                if od["ainc"]:
                    key = ("a", od["semkey"])
                    val = running[od["semkey"]]
                else:
                    if self._skip(od, o):
                        continue
                    key = ("e", od["eng"])
                    val = od["cnt"]
                waits[key] = max(waits.get(key, 0), val)
            o["waits"] = waits
            if o["ainc"]:
                running[o["semkey"]] = o["cnt"]
        seen = {e: {} for e in self.ENG}
        for o in ops:
            s = seen[o["eng"]]
            w2 = {}
            for k, v in o["waits"].items():
                if s.get(k, 0) >= v:
                    continue
                w2[k] = v
                s[k] = v
            o["waits"] = w2
        with ExitStack() as st:
            esem = {e: st.enter_context(nc.semaphore("s_" + e)) for e in self.ENG}
            asem = {k: st.enter_context(nc.semaphore("a_%d" % j)) for j, k in enumerate(a_cnt)}
            block = st.enter_context(nc.Block())

            def run_engine(ename):
                def body(e):
                    for o in ops:
                        if o["eng"] != ename:
                            continue
                        for (kind, k), v in o["waits"].items():
                            e.wait_ge(esem[k] if kind == "e" else asem[k], v)
                        if o["fn"] is None:
                            continue
                        ins = o["fn"](e)
                        if o["ainc"]:
                            ins.then_inc(asem[o["semkey"]], o["ainc"])
                        elif o["signal"]:
                            ins.then_inc(esem[ename], 1)
                    if ename == final_wait_eng:
                        for k, v in a_cnt.items():
                            e.wait_ge(asem[k], v)
                return body

            block.tensor(run_engine("pe"))
            block.scalar(run_engine("act"))
            block.vector(run_engine("dve"))
            block.gpsimd(run_engine("pool"))
            block.sync(run_engine("sp"))
        return len(a_cnt) + len(self.ENG)


class _Stop(Exception):
    pass


def build_program(NT, dbg=None, stop_after=None, lite=False, a1cut=99):
    nc = bass.Bass("TRN2", target_bir_lowering=False)
    NT128 = NT // 128
    NT256 = NT // 256
    NT512 = NT // 512
    NCH = NT // 64
    HALF = min(2048, NT)
    NHALF = NT // HALF

    def din(name, shape, dt=F32):
        if lite and name == "w_in":
            shape = [1, D, 4096]
        elif lite and name in ("w_proj_hgrn", "w_proj_conv", "w_out"):
            shape = [1, 128, 128]
        elif lite and name in ("dense_blob", "expert_blob"):
            shape = [1, 1, 128, 128]
        return nc.dram_tensor(name, list(shape), dt, kind="ExternalInput").ap()

    def dscr(name, shape, dt=F32):
        return nc.dram_tensor(name, list(shape), dt, kind="Internal").ap()

    x_in = din("x", [NT, D])
    w_in = din("w_in", [DEPTH, D, INW])
    lbnd = din("lower_bounds", [DEPTH, D])
    gnw_in = din("hgrn_norm_w", [DEPTH, 128, 1])
    convw_in = din("conv_w", [DEPTH, 128, 24])
    wph_in = din("w_proj_hgrn", [DEPTH, D, D])
    wpc_in = din("w_proj_conv", [DEPTH, D, D])
    wo_in = din("w_out", [DEPTH, D, D])
    nmix_in = din("norm_mix", [DEPTH, 128, 8])
    nffn_in = din("norm_ffn", [DEPTH, 128, 8])
    dblob_in = din("dense_blob", [1, DFF // 256, 128, 6144])
    rw_in = din("router_w", [128, 8, NE])
    eblob_in = din("expert_blob", [NE, DFF // 256, 128, 6144])
    fnorm_in = din("final_norm", [128, 8])
    c_ident = din("c_ident", [128, 128])
    c_triu = din("c_triu", [128, 128])
    c_tris = din("c_tris", [128, 128])
    c_mask = din("c_mask", [128, 8 * 64])
    c_sel = din("c_sel", [128, 1])
    out_d = nc.dram_tensor("out", [NT, D], F32, kind="ExternalOutput").ap()

    xT = dscr("xT", [8, 128, NT])
    qtT = dscr("qtT", [NT128, 128, 1024], BF16)
    ktT = dscr("ktT", [NT128, 128, 1024], BF16)
    khD = dscr("khD", [NT128, 128, 1024], BF16)
    vtD = dscr("vtD", [NT128, 128, 1024], BF16)
    gsT = dscr("gsT", [NT128, 128, 1024], BF16)
    opT = dscr("opT", [NT128, 128, 1024], F32)
    qbT = dscr("qbT", [NT128, 128, 1024], BF16)
    vcT = dscr("vcT", [8, 128, NT + 2], F32)
    ccT = dscr("ccT", [NT256, 128, 8 * 256], BF16)
    gaT = dscr("gaT", [NT256, 128, 8 * 256], BF16)
    gbT = dscr("gbT", [NT256, 128, 8 * 256], BF16)
    gin = dscr("gin", [1040, 128])
    gout = dscr("gout", [2080, 128])

    dbg_outs = {}
    if dbg:
        for name, shape in dbg.items():
            dbg_outs[name] = nc.dram_tensor("dbg_" + name, list(shape), F32, kind="ExternalOutput").ap()

    P = Prog(nc)
    with ExitStack() as gst:
        uniq = [0]

        def sb(name, shape, dt=F32, st=gst):
            uniq[0] += 1
            return st.enter_context(nc.sbuf_tensor("%s_%d" % (name, uniq[0]), list(shape), dt))

        ident = sb("ident", [128, 128])
        ident_bf = sb("ident_bf", [128, 128], BF16)
        ones = sb("ones", [128, 128])
        triu = sb("triu", [128, 128])
        tris = sb("tris", [128, 128])
        amask = sb("amask", [128, 8, 64])
        sel = sb("sel", [128, 1])
        gnw = sb("gnw", [128, DEPTH])
        convw = sb("convw", [128, DEPTH, 24])
        nmix = sb("nmix", [128, DEPTH, 8])
        nffn = sb("nffn", [128, DEPTH, 8])
        fnw = sb("fnw", [128, 8])
        rw = sb("rw", [128, 8, NE])
        totT = sb("totT", [128, 8, NCH])
        epsb = sb("epsb", [128, 1])
        PS = [gst.enter_context(nc.psum_tensor("ps%d" % i, [128, 1024], F32)) for i in range(4)]
        psrr = [0]

        def nps():
            i = psrr[0] % 4
            psrr[0] += 1
            return PS[i], "ps%d" % i

        C = "const"
        P.dma("sp", ident[:], c_ident[:, :], [], [C], "c0")
        P.dma("sp", triu[:], c_triu[:, :], [], [C], "c0")
        P.dma("sp", tris[:], c_tris[:, :], [], [C], "c0")
        P.dma("sp", amask[:].rearrange("p h t -> p (h t)"), c_mask[:, :], [], [C], "c0")
        P.dma("sp", sel[:], c_sel[:, :], [], [C], "c0")
        P.dma("sp", fnw[:], fnorm_in[:, :], [], [C], "c0")
        P.dma("sp", rw[:], rw_in[:, :, :], [], [C], "c0")
        for l in range(DEPTH):
            P.dma("sp", gnw[:, l:l + 1], gnw_in[l, :, :], [], [C], "c0")
            P.dma("sp", convw[:, l, :], convw_in[l, :, :], [], [C], "c0")
            P.dma("sp", nmix[:, l, :], nmix_in[l, :, :], [], [C], "c0")
            P.dma("sp", nffn[:, l, :], nffn_in[l, :, :], [], [C], "c0")
        P.add("pool", lambda e: e.memset(ones[:], 1.0), [], ["ones"])
        P.add("pool", lambda e: e.memset(epsb[:], EPS), [], ["epsb"])
        P.add("dve", lambda e: e.tensor_copy(out=ident_bf[:], in_=ident[:]), [C], ["identbf"])
        P.barrier()

        def chk(name):
            if stop_after == name:
                raise _Stop()

        try:
            with ExitStack() as st:
                xin = [sb("p0_x%d" % i, [128, 1024], st=st) for i in range(4)]
                xo = [sb("p0_o%d" % i, [128, 8, 128], st=st) for i in range(4)]
                for i in range(NT128):
                    b = i % 4
                    P.dma("sp", xin[b][:], x_in[i * 128:(i + 1) * 128, :], [], ["p0x%d" % b], "p0x%d" % b)
                    ps, pk = nps()
                    for dc in range(8):
                        P.add("pe", lambda e, ps=ps, b=b, dc=dc: e.transpose(
                            out=ps[:, dc * 128:(dc + 1) * 128], in_=xin[b][:, dc * 128:(dc + 1) * 128],
                            identity=ident[:]), ["p0x%d" % b, C], [pk])
                    P.add("dve", lambda e, ps=ps, b=b: e.tensor_copy(
                        out=xo[b][:].rearrange("p c t -> p (c t)"), in_=ps[:, :]), [pk], ["p0o%d" % b])
                    P.dma("pool", xT[:, :, i * 128:(i + 1) * 128].rearrange("c p t -> p c t"), xo[b][:],
                          ["p0o%d" % b], ["xT"], "p0o%d" % b)
            P.barrier()
            chk("p0")

            def emit_norm(xt, xk, w_ap, hout, hk, sq, sqk, rstd, rk, n=512, h32=None, h32k=None):
                P.add("act", lambda e: e.activation(out=sq, in_=xt, func=AF.Square), [xk], [sqk])
                ps, pk = nps()
                for dc in range(8):
                    P.add("pe", lambda e, dc=dc: e.matmul(ps[:, 0:n], lhsT=ones[:], rhs=sq[:, dc, :],
                                                          start=(dc == 0), stop=(dc == 7)),
                          [sqk, "ones"], [pk])
                P.add("act", lambda e: e.activation(out=rstd, in_=ps[:, 0:n], func=AF.Ln,
                                                    scale=1.0 / D, bias=epsb[:]), [pk, "epsb"], [rk])
                P.add("act", lambda e: e.activation(out=rstd, in_=rstd, func=AF.Exp, scale=-0.5), [rk], [rk])
                for dc in range(8):
                    tgt = hout if h32 is None else h32
                    P.add("dve", lambda e, dc=dc, tgt=tgt: e.scalar_tensor_tensor(
                        out=tgt[:, dc, :], in0=xt[:, dc, :], scalar=w_ap(dc), in1=rstd,
                        op0=ALU.mult, op1=ALU.mult), [xk, rk, C], [hk if h32 is None else h32k])
                if h32 is not None:
                    P.add("pool", lambda e: e.tensor_copy(out=hout, in_=h32), [h32k], [hk])

            def load_w_bf16(dst, src, key, nk, ncols):
                for kc in range(nk):
                    P.dma("pool", dst[:, kc, :], src[kc * 128:(kc + 1) * 128, :], [], [key], key)

            for l in range(DEPTH):
                with ExitStack() as st:
                    wA = sb("a1_w", [128, 8, 4096], BF16, st=st)
                    load_w_bf16(wA, w_in[l, :, 0:4096], "a1w", 8, 4096)
                    xt = [sb("a1_xt%d" % i, [128, 8, 512], st=st) for i in range(2)]
                    sq = sb("a1_sq", [128, 8, 512], st=st)
                    rstd = sb("a1_rstd", [128, 512], st=st)
                    hT2 = [sb("a1_hT%d" % i, [128, 8, 512], BF16, st=st) for i in range(2)]
                    f_sb = sb("a1_f", [128, 1024], st=st)
                    lbt_l = sb("a1_lbt", [128, 1024], st=st)
                    lbm_l = sb("a1_lbm", [128, 1024], st=st)
                    if l == 0:
                        P.add("dve", lambda e: e.memset(lbt_l[:], 0.0), [], ["a1lb"])
                        P.add("dve", lambda e: e.memset(lbm_l[:], 1.0), [], ["a1lb"])
                    else:
                        P.dma("sp", lbt_l[:], bass.AP(lbnd.tensor, l * D, [[0, 128], [1, D]]), [], ["a1lb"], "a1lb")
                        P.dma("sp", lbm_l[:], bass.AP(lbnd.tensor, 0, [[0, 128], [1, D]]), [], ["a1lb"], "a1lb")
                        P.add("dve", lambda e: e.tensor_sub(out=lbt_l[:], in0=lbt_l[:], in1=lbm_l[:]), ["a1lb"], ["a1lb"])
                        P.add("act", lambda e: e.activation(out=lbt_l[:], in_=lbt_l[:], func=AF.Sigmoid), ["a1lb"], ["a1lb"])
                        P.add("dve", lambda e: e.tensor_scalar(out=lbm_l[:], in0=lbt_l[:], scalar1=-1.0, scalar2=1.0,
                                                               op0=ALU.mult, op1=ALU.add), ["a1lb"], ["a1lb"])
                    k_sb = sb("a1_k", [128, 1024], st=st)
                    g_sb = sb("a1_g", [128, 1024], st=st)
                    e_sb = sb("a1_e", [128, 1024], st=st)
                    kt_sb = [sb("a1_kt%d" % i, [128, 1024], st=st) for i in range(2)]
                    e2_sb = sb("a1_e2", [128, 1024], st=st)
                    pending_tp = [None]
                    kh_sb = [sb("a1_kh%d" % i, [128, 1024], BF16, st=st) for i in range(2)]
                    ktT_sb = [sb("a1_ktT%d" % i, [128, 1024], BF16, st=st) for i in range(2)]
                    qt_sb = [sb("a1_qt%d" % i, [128, 1024], BF16, st=st) for i in range(2)]
                    vt_sb = [sb("a1_vt%d" % i, [128, 1024], BF16, st=st) for i in range(2)]
                    gs_sb = [sb("a1_gs%d" % i, [128, 1024], BF16, st=st) for i in range(2)]
                    ecT = sb("a1_ecT", [128, 1024], st=st)
                    ct_sb = sb("a1_ct", [128, 1024], st=st)
                    sq_q = sb("a1_sqq", [128, 1024], st=st)

                    def ld_x(j):
                        b = j % 2
                        P.dma("sp", xt[b][:], xT[:, :, j * 512:(j + 1) * 512].rearrange("c p t -> p c t"),
                              ["xT"], ["a1xt%d" % b], "a1xt%d" % b)
                    ld_x(0)
                    if NT512 > 1:
                        ld_x(1)

                    def norm1(j):
                        hb = j % 2
                        emit_norm(xt[hb][:], "a1xt%d" % hb, lambda dc: nmix[:, l, dc:dc + 1], hT2[hb][:], "a1hT%d" % hb,
                                  sq[:], "a1sq", rstd[:], "a1rstd")
                    norm1(0)
                    for j in range(NT512):
                        b = j % 2
                        hT = hT2[b]
                        hk = "a1hT%d" % b
                        for s in range(4):
                            if s == 2:
                                if j + 1 < NT512:
                                    norm1(j + 1)
                                if j + 2 < NT512:
                                    ld_x(j + 2)
                            i = j * 4 + s
                            ob = i % 2
                            hs = hT[:, :, s * 128:(s + 1) * 128]
                            kb = i % 2
                            zp, zk = nps()
                            for hf in range(2):
                                for kc in range(8):
                                    P.add("pe", lambda e, hf=hf, kc=kc, zp=zp, hs=hs: e.matmul(
                                        zp[:, hf * 512:(hf + 1) * 512], lhsT=hs[:, kc, :],
                                        rhs=wA[:, kc, 1024 + hf * 512:1024 + (hf + 1) * 512],
                                        start=(kc == 0), stop=(kc == 7)), [hk, "a1w"], [zk])
                            P.add("act", lambda e, zp=zp: e.activation(out=f_sb[:], in_=zp[:, :], func=AF.Sigmoid),
                                  [zk], ["a1f"])
                            P.add("dve", lambda e: e.tensor_mul(out=f_sb[:], in0=f_sb[:], in1=lbm_l[:]),
                                  ["a1f", "a1lb"], ["a1f"])
                            P.add("dve", lambda e: e.tensor_add(out=f_sb[:], in0=f_sb[:], in1=lbt_l[:]),
                                  ["a1f", "a1lb"], ["a1f"])
                            P.add("dve", lambda e: e.tensor_scalar(out=k_sb[:], in0=f_sb[:], scalar1=-1.0, scalar2=1.0,
                                                                   op0=ALU.mult, op1=ALU.add), ["a1f"], ["a1k"])
                            P.add("dve", lambda e: e.tensor_scalar_max(out=f_sb[:], in0=f_sb[:], scalar1=F_MIN),
                                  ["a1f"], ["a1f"])
                            qp, qk = nps()
                            for h in range(8):
                                for kc in range(8):
                                    P.add("pe", lambda e, h=h, kc=kc, qp=qp, hs=hs: e.matmul(
                                        qp[:, h * 128:(h + 1) * 128], lhsT=wA[:, kc, h * 128:(h + 1) * 128],
                                        rhs=hs[:, kc, :], start=(kc == 0), stop=(kc == 7)), [hk, "a1w"], [qk])
                            P.add("act", lambda e, qp=qp: e.activation(out=sq_q[:], in_=qp[:, :], func=AF.Silu),
                                  [qk], ["a1sqq"])
                            if pending_tp[0] is not None:
                                pending_tp[0]()
                                pending_tp[0] = None
                            gp, gk = nps()
                            for h in range(8):
                                for kc in range(8):
                                    P.add("pe", lambda e, h=h, kc=kc, gp=gp, hs=hs: e.matmul(
                                        gp[:, h * 128:(h + 1) * 128], lhsT=wA[:, kc, 3072 + h * 128:3072 + (h + 1) * 128],
                                        rhs=hs[:, kc, :], start=(kc == 0), stop=(kc == 7)), [hk, "a1w"], [gk])
                            P.add("act", lambda e, gp=gp, ob=ob: e.activation(out=gs_sb[ob][:], in_=gp[:, :], func=AF.Silu),
                                  [gk], ["a1gs%d" % ob])
                            P.dma("pool", gsT[i, :, :], gs_sb[ob][:], ["a1gs%d" % ob], ["gsT"], "a1gs%d" % ob)
                            vp, vk = nps()
                            for hf in range(2):
                                for kc in range(8):
                                    P.add("pe", lambda e, hf=hf, kc=kc, vp=vp, hs=hs: e.matmul(
                                        vp[:, hf * 512:(hf + 1) * 512], lhsT=hs[:, kc, :],
                                        rhs=wA[:, kc, 2048 + hf * 512:2048 + (hf + 1) * 512],
                                        start=(kc == 0), stop=(kc == 7)), [hk, "a1w"], [vk])
                            P.add("act", lambda e, vp=vp, ob=ob: e.copy(out=vt_sb[ob][:], in_=vp[:, :]),
                                  [vk], ["a1vt%d" % ob])
                            P.dma("pool", vtD[i, :, :], vt_sb[ob][:], ["a1vt%d" % ob], ["vtD"], "a1vt%d" % ob)
                            P.add("act", lambda e: e.activation(out=g_sb[:], in_=f_sb[:], func=AF.Ln), ["a1f"], ["a1g"])
                            cp, ck = nps()
                            npz, nk_ = nps()
                            for hf in range(2):
                                P.add("pe", lambda e, hf=hf, cp=cp: e.matmul(
                                    cp[:, hf * 512:(hf + 1) * 512], lhsT=triu[:], rhs=g_sb[:, hf * 512:(hf + 1) * 512],
                                    start=True, stop=True), ["a1g", C], [ck])
                                P.add("pe", lambda e, hf=hf, npz=npz: e.matmul(
                                    npz[:, hf * 512:(hf + 1) * 512], lhsT=tris[:], rhs=g_sb[:, hf * 512:(hf + 1) * 512],
                                    start=True, stop=True), ["a1g", C], [nk_])
                            ctp, ctk = nps()
                            for h in range(8):
                                P.add("pe", lambda e, h=h, ctp=ctp: e.matmul(
                                    ctp[:, h * 128:(h + 1) * 128], lhsT=g_sb[:, h * 128:(h + 1) * 128], rhs=triu[:],
                                    start=True, stop=True), ["a1g", C], [ctk])
                            P.add("act", lambda e, cp=cp: e.activation(out=e_sb[:], in_=cp[:, :], func=AF.Exp, scale=-1.0),
                                  [ck], ["a1e"])
                            P.add("dve", lambda e, kb=kb: e.tensor_mul(out=kt_sb[kb][:], in0=k_sb[:], in1=e_sb[:]),
                                  ["a1k", "a1e"], ["a1kt%d" % kb])
                            P.add("act", lambda e, npz=npz: e.activation(out=e2_sb[:], in_=npz[:, :], func=AF.Exp),
                                  [nk_], ["a1e2"])
                            P.add("dve", lambda e, ob=ob: e.tensor_mul(out=kh_sb[ob][:], in0=k_sb[:], in1=e2_sb[:]),
                                  ["a1k", "a1e2"], ["a1kh%d" % ob])
                            P.dma("pool", khD[i, :, :], kh_sb[ob][:], ["a1kh%d" % ob], ["khD"], "a1kh%d" % ob)
                            P.add("dve", lambda e, ctp=ctp: e.tensor_copy(out=ct_sb[:], in_=ctp[:, :]), [ctk], ["a1ct"])
                            P.add("act", lambda e: e.activation(out=ecT[:], in_=ct_sb[:], func=AF.Exp),
                                  ["a1ct"], ["a1ecT"])
                            for c2 in range(2):
                                P.add("pool", lambda e, i=i, c2=c2: e.tensor_copy(
                                    out=totT[:, :, 2 * i + c2],
                                    in_=ct_sb[:].rearrange("p (h t) -> p h t", h=8)[:, :, c2 * 64 + 63]),
                                    ["a1ct"], ["totT"])
                            P.add("dve", lambda e, ob=ob: e.scalar_tensor_tensor(
                                out=qt_sb[ob][:], in0=sq_q[:], scalar=SCALE, in1=ecT[:], op0=ALU.mult, op1=ALU.mult),
                                ["a1sqq", "a1ecT"], ["a1qt%d" % ob])
                            P.dma("pool", qtT[i, :, :], qt_sb[ob][:], ["a1qt%d" % ob], ["qtT"], "a1qt%d" % ob)

                            def do_tp(i=i, ob=ob, kb=kb):
                                tp, tk = nps()
                                for h in range(8):
                                    P.add("pe", lambda e, h=h, tp=tp: e.transpose(
                                        out=tp[:, h * 128:(h + 1) * 128], in_=kt_sb[kb][:, h * 128:(h + 1) * 128],
                                        identity=ident[:]), ["a1kt%d" % kb, C], [tk])
                                P.add("dve", lambda e, tp=tp: e.tensor_copy(out=ktT_sb[ob][:], in_=tp[:, :]),
                                      [tk], ["a1ktT%d" % ob])
                                P.dma("pool", ktT[i, :, :], ktT_sb[ob][:], ["a1ktT%d" % ob], ["ktT"], "a1ktT%d" % ob)
                            pending_tp[0] = do_tp
                    if pending_tp[0] is not None:
                        pending_tp[0]()
                        pending_tp[0] = None
                P.barrier()
                chk("a1_%d" % l)

                with ExitStack() as st:
                    wB = sb("a2_w", [128, 8, 5120], BF16, st=st)
                    load_w_bf16(wB, w_in[l, :, 4096:9216], "a2w", 8, 5120)
                    xt = [sb("a2_xt%d" % i, [128, 8, 256], st=st) for i in range(2)]
                    sq2 = [sb("a2_sq%d" % i, [128, 8, 256], st=st) for i in range(2)]
                    rstd2 = [sb("a2_rstd%d" % i, [128, 256], st=st) for i in range(2)]
                    hT2 = [sb("a2_hT%d" % i, [128, 8, 256], BF16, st=st) for i in range(2)]
                    b_sb = sb("a2_b", [128, 8, 256], st=st)
                    vc_sb = [sb("a2_vc%d" % i, [128, 8, 256], st=st) for i in range(2)]
                    c_sb = [sb("a2_c%d" % i, [128, 8 * 256], BF16, st=st) for i in range(2)]
                    ga_sb = [sb("a2_ga%d" % i, [128, 8 * 256], BF16, st=st) for i in range(2)]
                    gb_sb = [sb("a2_gb%d" % i, [128, 8 * 256], BF16, st=st) for i in range(2)]

                    def ld_x2(j):
                        b = j % 2
                        P.dma("sp", xt[b][:], xT[:, :, j * 256:(j + 1) * 256].rearrange("c p t -> p c t"),
                              ["xT"], ["a2xt%d" % b], "a2xt%d" % b)
                    ld_x2(0)
                    if NT256 > 1:
                        ld_x2(1)

                    def norm2(j):
                        hb = j % 2
                        emit_norm(xt[hb][:], "a2xt%d" % hb, lambda dc: nmix[:, l, dc:dc + 1], hT2[hb][:], "a2hT%d" % hb,
                                  sq2[hb][:], "a2sq%d" % hb, rstd2[hb][:], "a2rstd%d" % hb, n=256)
                    norm2(0)

                    def proj2(col0, j):
                        res = []
                        for hf in range(2):
                            ps, pk = nps()
                            for c4 in range(4):
                                cc = hf * 4 + c4
                                for kc in range(8):
                                    P.add("pe", lambda e, ps=ps, c4=c4, cc=cc, kc=kc: e.matmul(
                                        ps[:, c4 * 256:(c4 + 1) * 256],
                                        lhsT=wB[:, kc, col0 + cc * 128:col0 + (cc + 1) * 128],
                                        rhs=hT2[j % 2][:, kc, :], start=(kc == 0), stop=(kc == 7)),
                                        ["a2hT%d" % (j % 2), "a2w"], [pk])
                            res.append((ps, pk))
                        return res

                    for j in range(NT256):
                        b = j % 2
                        if j + 1 < NT256:
                            norm2(j + 1)
                        if j + 2 < NT256:
                            ld_x2(j + 2)
                        for hf, (ps, pk) in enumerate(proj2(0, j)):
                            P.add("act", lambda e, ps=ps, hf=hf: e.copy(
                                out=b_sb[:, hf * 4:(hf + 1) * 4, :].rearrange("p c t -> p (c t)"), in_=ps[:, :]),
                                [pk], ["a2b"])
                        for hf, (ps, pk) in enumerate(proj2(1024, j)):
                            P.add("act", lambda e, ps=ps, hf=hf, b=b: e.copy(
                                out=c_sb[b][:, hf * 1024:(hf + 1) * 1024], in_=ps[:, :]), [pk], ["a2c%d" % b])
                        P.dma("pool", ccT[j, :, :], c_sb[b][:], ["a2c%d" % b], ["ccT"], "a2c%d" % b)
                        for hf, (ps, pk) in enumerate(proj2(2048, j)):
                            P.add("dve", lambda e, ps=ps, hf=hf, b=b: e.tensor_mul(
                                out=vc_sb[b][:, hf * 4:(hf + 1) * 4, :].rearrange("p c t -> p (c t)"),
                                in0=b_sb[:, hf * 4:(hf + 1) * 4, :].rearrange("p c t -> p (c t)"), in1=ps[:, :]),
                                [pk, "a2b"], ["a2vc%d" % b])
                        P.dma("pool", vcT[:, :, 2 + j * 256:2 + (j + 1) * 256].rearrange("c p t -> p c t"), vc_sb[b][:],
                              ["a2vc%d" % b], ["vcT"], "a2vc%d" % b)
                        for hf, (ps, pk) in enumerate(proj2(3072, j)):
                            P.add("act", lambda e, ps=ps, hf=hf, b=b: e.activation(
                                out=ga_sb[b][:, hf * 1024:(hf + 1) * 1024], in_=ps[:, :], func=AF.Sigmoid),
                                [pk], ["a2ga%d" % b])
                        P.dma("pool", gaT[j, :, :], ga_sb[b][:], ["a2ga%d" % b], ["gaT"], "a2ga%d" % b)
                        for hf, (ps, pk) in enumerate(proj2(4096, j)):
                            P.add("act", lambda e, ps=ps, hf=hf, b=b: e.activation(
                                out=gb_sb[b][:, hf * 1024:(hf + 1) * 1024], in_=ps[:, :], func=AF.Sigmoid),
                                [pk], ["a2gb%d" % b])
                        P.dma("pool", gbT[j, :, :], gb_sb[b][:], ["a2gb%d" % b], ["gbT"], "a2gb%d" % b)
                P.barrier()
                chk("a2_%d" % l)

                with ExitStack() as st:
                    S32 = sb("b_S32", [128, 8, 128], st=st)
                    SR = 4
                    Sbf = [sb("b_Sbf%d" % i, [128, 8, 128], BF16, st=st) for i in range(SR)]
                    Gp = sb("b_G", [128, 8], st=st)
                    EG = sb("b_EG", [128, 8], st=st)
                    dk = sb("b_dk", [128, 8, NCH], st=st)
                    qt = [sb("b_qt%d" % i, [128, 8, 128], BF16, st=st) for i in range(2)]
                    kt = [sb("b_kt%d" % i, [128, 8, 128], BF16, st=st) for i in range(2)]
                    kh = [sb("b_kh%d" % i, [128, 1024], BF16, st=st) for i in range(2)]
                    vt = [sb("b_vt%d" % i, [128, 1024], BF16, st=st) for i in range(2)]
                    at_sb = [sb("b_at%d" % i, [128, 8, 64], BF16, st=st) for i in range(2)]
                    o_sb = [sb("b_o%d" % i, [128, 1024], st=st) for i in range(2)]
                    qb_sb = [sb("b_qb%d" % i, [128, 8, 128], BF16, st=st) for i in range(2)]
                    P.add("dve", lambda e: e.memset(S32[:], 0.0), [], ["bS32_%d" % hh for hh in range(8)])
                    P.add("dve", lambda e: e.memset(Sbf[0][:], 0.0), [], ["bSbf0"])
                    P.add("dve", lambda e: e.memset(Gp[:], 0.0), [], ["bG"])
                    P.add("dve", lambda e: e.memset(EG[:], 1.0), [], ["bEG"])
                    P.add("act", lambda e: e.activation(out=dk[:], in_=totT[:], func=AF.Exp), ["totT"], ["bdk"])

                    def ld_b(i):
                        b = i % 2
                        P.dma("sp", qt[b][:].rearrange("p h t -> p (h t)"), qtT[i, :, :], ["qtT"], ["bqt%d" % b], "bqt%d" % b)
                        P.dma("sp", kt[b][:].rearrange("p h t -> p (h t)"), ktT[i, :, :], ["ktT"], ["bkt%d" % b], "bkt%d" % b)
                        P.dma("sp", kh[b][:], khD[i, :, :], ["khD"], ["bkh%d" % b], "bkh%d" % b)
                        P.dma("sp", vt[b][:], vtD[i, :, :], ["vtD"], ["bvt%d" % b], "bvt%d" % b)
                    ld_b(0)
                    for i in range(NT128):
                        b = i % 2
                        if i + 1 < NT128:
                            ld_b(i + 1)
                        ab2 = i % 2
                        ap_, ak = nps()
                        for c in range(2):
                            for h in range(8):
                                P.add("pe", lambda e, c=c, h=h, ap_=ap_, b=b: e.matmul(
                                    ap_[c * 64:(c + 1) * 64, h * 64:(h + 1) * 64],
                                    lhsT=kt[b][:, h, c * 64:(c + 1) * 64], rhs=qt[b][:, h, c * 64:(c + 1) * 64],
                                    start=True, stop=True), ["bkt%d" % b, "bqt%d" % b], [ak])
                        P.add("dve", lambda e, ap_=ap_, ab2=ab2: e.tensor_mul(
                            out=at_sb[ab2][:].rearrange("p h t -> p (h t)"), in0=ap_[:, 0:512],
                            in1=amask[:].rearrange("p h t -> p (h t)")), [ak, C], ["bat%d" % ab2])
                        ups = []
                        for c in range(2):
                            up, uk = nps()
                            for h in range(8):
                                P.add("pe", lambda e, c=c, h=h, up=up, b=b: e.matmul(
                                    up[:, h * 128:(h + 1) * 128],
                                    lhsT=kh[b][c * 64:(c + 1) * 64, h * 128:(h + 1) * 128],
                                    rhs=vt[b][c * 64:(c + 1) * 64, h * 128:(h + 1) * 128], start=True, stop=True),
                                    ["bkh%d" % b, "bvt%d" % b], [uk])
                            ups.append((up, uk))
                        op_, ok_ = nps()
                        for c in range(2):
                            ch = 2 * i + c
                            r0, r1 = ch % SR, (ch + 1) % SR
                            P.add("dve", lambda e, c=c, b=b: e.tensor_tensor(
                                out=qb_sb[b][:, :, c * 64:(c + 1) * 64], in0=qt[b][:, :, c * 64:(c + 1) * 64],
                                in1=EG[:].unsqueeze(2).broadcast_to([128, 8, 64]), op=ALU.mult),
                                ["bqt%d" % b, "bEG"], ["bqb%d_%d" % (b, c)])
                            P.add("dve", lambda e, ch=ch: e.tensor_add(out=Gp[:], in0=Gp[:], in1=totT[:, :, ch]),
                                  ["bG", "totT"], ["bG"])
                            P.add("act", lambda e: e.activation(out=EG[:], in_=Gp[:], func=AF.Exp), ["bG"], ["bEG"])
                            for h in range(8):
                                P.add("pe", lambda e, c=c, h=h, op_=op_, b=b, ab2=ab2: e.matmul(
                                    op_[:, h * 128 + c * 64:h * 128 + (c + 1) * 64],
                                    lhsT=vt[b][c * 64:(c + 1) * 64, h * 128:(h + 1) * 128],
                                    rhs=at_sb[ab2][c * 64:(c + 1) * 64, h, :], start=True, stop=False),
                                    ["bvt%d" % b, "bat%d" % ab2], [ok_])
                                P.add("pe", lambda e, c=c, h=h, op_=op_, b=b, r0=r0: e.matmul(
                                    op_[:, h * 128 + c * 64:h * 128 + (c + 1) * 64],
                                    lhsT=Sbf[r0][:, h, :], rhs=qt[b][:, h, c * 64:(c + 1) * 64], start=False, stop=True),
                                    ["bSbf%d" % r0, "bqt%d" % b], [ok_])
                            up, uk = ups[c]
                            for h in range(8):
                                P.add("dve", lambda e, h=h, up=up, ch=ch: e.scalar_tensor_tensor(
                                    out=S32[:, h, :], in0=S32[:, h, :], scalar=dk[:, h, ch:ch + 1],
                                    in1=up[:, h * 128:(h + 1) * 128], op0=ALU.mult, op1=ALU.add),
                                    ["bS32_%d" % h, "bdk", uk], ["bS32_%d" % h])
                            P.add("act", lambda e, r1=r1: e.copy(out=Sbf[r1][:], in_=S32[:]),
                                  ["bS32_%d" % hh for hh in range(8)], ["bSbf%d" % r1])
                        P.add("act", lambda e, op_=op_, b=b: e.copy(out=o_sb[b][:], in_=op_[:, :]), [ok_], ["bo%d" % b])
                        P.dma("pool", opT[i, :, :], o_sb[b][:], ["bo%d" % b], ["opT"], "bo%d" % b)
                        P.dma("pool", qbT[i, :, :], qb_sb[b][:].rearrange("p h t -> p (h t)"), ["bqb%d_%d" % (b, hh) for hh in range(2)], ["qbT"], "bqb%d" % b)
                    P.dma("pool", gin[0:1024, :].rearrange("(h k) v -> k h v", h=8), S32[:], ["bS32_%d" % hh for hh in range(8)], ["gin"], "bgin")
                    P.dma("pool", gin[1024:1040, :].rearrange("(c r) (q t) -> c (r q) t", c=8, t=2),
                          vcT[:, :, NT:NT + 2], ["vcT"], ["gin"], "bgin")
                P.barrier()
                chk("b_%d" % l)
                P.add("pool", lambda e: e.collective_compute(
                    "AllGather", ALU.bypass, replica_groups=[[2 * i, 2 * i + 1] for i in range(NCORES // 2)],
                    ins=[gin[:, :]], outs=[gout[:, :]]), ["gin"], ["gout"], ainc=1, semkey="cc%d" % l)
                P.barrier()
                chk("cc_%d" % l)

                with ExitStack() as st:
                    wph = sb("c_wph", [128, 8, 1024], BF16, st=st)
                    wpc = sb("c_wpc", [128, 8, 1024], BF16, st=st)
                    wo = sb("c_wo", [128, 8, 1024], BF16, st=st)
                    load_w_bf16(wph, wph_in[l], "cw", 8, 1024)
                    load_w_bf16(wpc, wpc_in[l], "cw", 8, 1024)
                    load_w_bf16(wo, wo_in[l], "cw", 8, 1024)
                    SA32 = sb("c_SA32", [128, 8, 128], st=st)
                    SAbf = sb("c_SAbf", [128, 8, 128], BF16, st=st)
                    halo = sb("c_halo", [128, 8, 2], st=st)
                    P.dma("sp", SA32[:], gout[0:1024, :].rearrange("(h k) v -> k h v", h=8), ["gout"], ["cSA32"], "cSA")
                    P.dma("sp", halo[:], gout[1024:1040, :].rearrange("(c r) (q t) -> (r q) c t", c=8, t=2),
                          ["gout"], ["chalo"], "cSA")
                    P.add("dve", lambda e: e.tensor_scalar(out=SAbf[:], in0=SA32[:], scalar1=sel[:, 0:1], scalar2=None,
                                                           op0=ALU.mult), ["cSA32", C], ["cSAbf"])
                    P.add("dve", lambda e: e.tensor_scalar(out=halo[:], in0=halo[:], scalar1=sel[:, 0:1], scalar2=None,
                                                           op0=ALU.mult), ["chalo", C], ["chalo"])
                    P.dma("sp", vcT[:, :, 0:2].rearrange("c p t -> p c t"), halo[:], ["chalo"], ["vcT"], "chalo")
                    N = 128
                    N2 = 256
                    NB = 3
                    NP = NT // N2
                    op_sb = [sb("c_op%d" % i, [128, 1024], st=st) for i in range(NB)]
                    qb_sb = [sb("c_qb%d" % i, [128, 1024], BF16, st=st) for i in range(NB)]
                    gs_sb = [sb("c_gs%d" % i, [128, 1024], BF16, st=st) for i in range(NB)]
                    vc_sb = [sb("c_vc%d" % i, [128, 8, N + 2], st=st) for i in range(NB)]
                    cc_sb = [sb("c_cc%d" % i, [128, 8, N], BF16, st=st) for i in range(NB)]
                    ga_sb = [sb("c_ga%d" % i, [128, 8 * N2], BF16, st=st) for i in range(2)]
                    gb_sb = [sb("c_gb%d" % i, [128, 8 * N2], BF16, st=st) for i in range(2)]
                    xt_sb = [sb("c_xt%d" % i, [128, 8, N2], st=st) for i in range(2)]
                    sqo2 = [sb("c_sqo%d" % i, [128, 1024], st=st) for i in range(2)]
                    rstd2 = [sb("c_rstd%d" % i, [128, 1024], st=st) for i in range(2)]
                    on_bf = [sb("c_on%d" % i, [128, 8, N2], BF16, st=st) for i in range(2)]
                    cdiag = sb("c_cdiag", [128, 24, 128], st=st)
                    for jc in range(24):
                        P.add("dve", lambda e, jc=jc: e.tensor_scalar(
                            out=cdiag[:, jc, :], in0=ident[:], scalar1=convw[:, l, jc:jc + 1], scalar2=None,
                            op0=ALU.mult), [C], ["cdiag"])
                    yc_bf = [sb("c_yc%d" % i, [128, 8, N2], BF16, st=st) for i in range(2)]
                    m1 = sb("c_m1", [128, 8 * N2], st=st)
                    m2 = sb("c_m2", [128, 8 * N2], st=st)
                    mg_bf = [sb("c_mg%d" % i, [128, 8, N2], BF16, st=st) for i in range(2)]

                    def ld_t(i):
                        b = i % NB
                        j2, s2 = i // 2, i % 2
                        P.dma("sp", op_sb[b][:], opT[i, :, :], ["opT"], ["cop%d" % b], "cld%d" % b)
                        P.dma("sp", qb_sb[b][:], qbT[i, :, :], ["qbT"], ["cqb%d" % b], "cld%d" % b)
                        P.dma("sp", gs_sb[b][:], gsT[i, :, :], ["gsT"], ["cgs%d" % b], "cld%d" % b)
                        P.dma("sp", vc_sb[b][:], vcT[:, :, i * N:(i + 1) * N + 2].rearrange("c p t -> p c t"),
                              ["vcT"], ["cvc%d" % b], "cld%d" % b)
                        P.dma("sp", cc_sb[b][:],
                              ccT[j2, :, :].rearrange("p (c t) -> p c t", c=8)[:, :, s2 * 128:(s2 + 1) * 128],
                              ["ccT"], ["ccc%d" % b], "cld%d" % b)

                    def ld_g(p):
                        b = p % 2
                        P.dma("sp", ga_sb[b][:], gaT[p, :, :], ["gaT"], ["cga%d" % b], "clg%d" % b)
                        P.dma("sp", gb_sb[b][:], gbT[p, :, :], ["gbT"], ["cgb%d" % b], "clg%d" % b)

                    def ld_x(p):
                        b = p % 2
                        P.dma("sp", xt_sb[b][:], xT[:, :, p * N2:(p + 1) * N2].rearrange("c p t -> p c t"),
                              ["xTc%d" % p], ["cxt%d" % b], "clx%d" % b)

                    def mmpair(w, rhs, rkeys, wk):
                        res = []
                        for hf in range(2):
                            ps, pk = nps()
                            for d4 in range(4):
                                dc = hf * 4 + d4
                                for kc in range(8):
                                    P.add("pe", lambda e, ps=ps, d4=d4, dc=dc, kc=kc: e.matmul(
                                        ps[:, d4 * N2:(d4 + 1) * N2], lhsT=w[:, kc, dc * 128:(dc + 1) * 128],
                                        rhs=rhs[:, kc, :], start=(kc == 0), stop=(kc == 7)), list(rkeys) + [wk], [pk])
                            res.append((ps, pk))
                        return res

                    def stage_h1(i):
                        b = i % NB
                        pp = (i // 2) % 2
                        hs = i % 2
                        csl = slice(hs * N, (hs + 1) * N)
                        sqo, rstd = sqo2[hs], rstd2[hs]
                        sqk, rsk = "csqo%d" % hs, "crstd%d" % hs
                        ps, pk = nps()
                        for h in range(8):
                            P.add("pe", lambda e, ps=ps, h=h, b=b: e.matmul(
                                ps[:, h * N:(h + 1) * N], lhsT=SAbf[:, h, :], rhs=qb_sb[b][:, h * N:(h + 1) * N],
                                start=True, stop=True), ["cSAbf", "cqb%d" % b], [pk])
                        P.add("dve", lambda e, ps=ps, b=b: e.tensor_add(out=op_sb[b][:], in0=op_sb[b][:], in1=ps[:, :]),
                              [pk, "cop%d" % b], ["cop%d" % b])
                        P.add("act", lambda e, b=b: e.activation(out=sqo[:], in_=op_sb[b][:], func=AF.Square),
                              ["cop%d" % b], [sqk])
                        ps, pk = nps()
                        for h in range(8):
                            P.add("pe", lambda e, ps=ps, h=h: e.matmul(
                                ps[:, h * N:(h + 1) * N], lhsT=ones[:], rhs=sqo[:, h * N:(h + 1) * N],
                                start=True, stop=True), [sqk, "ones"], [pk])
                        P.add("act", lambda e, ps=ps: e.activation(out=rstd[:], in_=ps[:, :], func=AF.Ln,
                                                                   scale=1.0 / 128, bias=epsb[:]), [pk, "epsb"], [rsk])
                        P.add("act", lambda e: e.activation(out=rstd[:], in_=rstd[:], func=AF.Exp, scale=-0.5),
                              [rsk], [rsk])
                        P.add("dve", lambda e, b=b: e.scalar_tensor_tensor(
                            out=sqo[:], in0=op_sb[b][:], scalar=gnw[:, l:l + 1], in1=rstd[:],
                            op0=ALU.mult, op1=ALU.mult), ["cop%d" % b, rsk, C, sqk], [sqk])
                        P.add("dve", lambda e, b=b, pp=pp, csl=csl: e.tensor_mul(
                            out=on_bf[pp][:, :, csl], in0=sqo[:].rearrange("p (h t) -> p h t", h=8),
                            in1=gs_sb[b][:].rearrange("p (h t) -> p h t", h=8)),
                            [sqk, "cgs%d" % b], ["con%d_%d" % (pp, hs)])
                        cps, cpk = nps()
                        for cc in range(8):
                            for j in range(3):
                                P.add("pe", lambda e, cps=cps, cc=cc, j=j, b=b: e.matmul(
                                    cps[:, cc * N:(cc + 1) * N], lhsT=cdiag[:, j * 8 + cc, :],
                                    rhs=vc_sb[b][:, cc, j:N + j], start=(j == 0), stop=(j == 2)),
                                    ["cvc%d" % b, "cdiag"], [cpk])
                        P.add("dve", lambda e, cps=cps, b=b, pp=pp, csl=csl: e.tensor_mul(
                            out=yc_bf[pp][:, :, csl], in0=cps[:, :].rearrange("p (c t) -> p c t", c=8), in1=cc_sb[b][:]),
                            [cpk, "ccc%d" % b], ["cyc%d_%d" % (pp, hs)])

                    def stage_h2a(p):
                        b = p % 2
                        ya = mmpair(wph, on_bf[b], ["con%d_0" % b, "con%d_1" % b], "cw")
                        for hf in range(2):
                            pa, pak = ya[hf]
                            P.add("dve", lambda e, pa=pa, b=b, hf=hf: e.tensor_mul(
                                out=m1[:, hf * 4 * N2:(hf + 1) * 4 * N2], in0=pa[:, :],
                                in1=ga_sb[b][:, hf * 4 * N2:(hf + 1) * 4 * N2]), [pak, "cga%d" % b], ["cm1_%d" % hf])
                        yb = mmpair(wpc, yc_bf[b], ["cyc%d_0" % b, "cyc%d_1" % b], "cw")
                        for hf in range(2):
                            pb, pbk = yb[hf]
                            P.add("dve", lambda e, pb=pb, b=b, hf=hf: e.tensor_mul(
                                out=m2[:, hf * 4 * N2:(hf + 1) * 4 * N2], in0=pb[:, :],
                                in1=gb_sb[b][:, hf * 4 * N2:(hf + 1) * 4 * N2]), [pbk, "cgb%d" % b], ["cm2_%d" % hf])
                        P.add("dve", lambda e, b=b: e.tensor_add(
                            out=mg_bf[b][:].rearrange("p c t -> p (c t)"), in0=m1[:], in1=m2[:]),
                            ["cm1_0", "cm1_1", "cm2_0", "cm2_1"], ["cmg%d" % b])

                    def stage_h2b(p):
                        b = p % 2
                        yo = mmpair(wo, mg_bf[b], ["cmg%d" % b], "cw")
                        for hf in range(2):
                            po, pok = yo[hf]
                            P.add("dve", lambda e, po=po, b=b, hf=hf: e.tensor_add(
                                out=xt_sb[b][:, hf * 4:(hf + 1) * 4, :].rearrange("p c t -> p (c t)"),
                                in0=xt_sb[b][:, hf * 4:(hf + 1) * 4, :].rearrange("p c t -> p (c t)"), in1=po[:, :]),
                                [pok, "cxt%d" % b], ["cxt%d" % b])
                        P.dma("pool", xT[:, :, p * N2:(p + 1) * N2].rearrange("c p t -> p c t"), xt_sb[b][:],
                              ["cxt%d" % b], ["xTc%d" % p], "cxts%d" % b)

                    ld_t(0)
                    ld_t(1)
                    ld_g(0)
                    ld_x(0)
                    if NP > 1:
                        ld_x(1)
                    for p in range(NP + 2):
                        if p < NP:
                            stage_h1(2 * p)
                            stage_h1(2 * p + 1)
                            if 2 * p + 2 < NT128:
                                ld_t(2 * p + 2)
                                ld_t(2 * p + 3)
                        if 0 <= p - 1 < NP:
                            stage_h2a(p - 1)
                        if 0 <= p - 2 < NP:
                            stage_h2b(p - 2)
                        if p + 1 < NP:
                            ld_g(p + 1)
                        if p >= 2 and p < NP:
                            ld_x(p)
                P.barrier()
                chk("c_%d" % l)
                if dbg and ("xT_mix%d" % l) in dbg_outs:
                    P.dma("sp", dbg_outs["xT_mix%d" % l].rearrange("(c p) t -> c p t", p=128), xT[:, :, :], ["xT"], [], "dbg")
                    P.barrier()

                moe = (l % 2 == 1)
                nexp = NE if moe else 1
                WB = eblob_in if moe else dblob_in
                with ExitStack() as st:
                    acc = sb("f_acc", [128, 8, HALF], st=st)
                    hT = sb("f_hT", [128, 8, HALF], BF16, st=st)
                    sq = sb("f_sq", [128, 8, 512], st=st)
                    rstd = sb("f_rstd", [128, 512], st=st)
                    wt = [sb("f_wt%d" % i, [128, 6144], BF16, st=st) for i in range(2)]
                    sa = [sb("f_sa%d" % i, [128, 512], st=st) for i in range(2)]
                    pr = [sb("f_pr%d" % i, [128, 512], st=st) for i in range(2)]
                    act = [sb("f_act%d" % i, [128, 512], BF16, st=st) for i in range(4)]
                    if moe:
                        gbc = sb("f_gbc", [128, NE, HALF], BF16, st=st)
                        lg = sb("f_lg", [128, 4, NE], st=st)
                        l2 = sb("f_l2", [128, 4, NE], st=st)
                        mx = sb("f_mx", [128, 2, 4], st=st)
                        msk = sb("f_msk", [128, 4, NE], st=st)
                        gt = sb("f_gt", [128, 4, NE], st=st)
                        den = sb("f_den", [128, 4], st=st)
                        gexp2 = [sb("f_gexp%d" % i, [128, NE, 128], st=st) for i in range(2)]
                    for hv in range(NHALF):
                        t0 = hv * HALF
                        for tt in range(HALF // 512):
                            P.dma("sp", acc[:, :, tt * 512:(tt + 1) * 512],
                                  xT[:, :, t0 + tt * 512:t0 + (tt + 1) * 512].rearrange("c p t -> p c t"),
                                  ["xT"], ["facc%d" % tt], "facc%d" % tt)
                        for tt in range(HALF // 512):
                            tsl = slice(tt * 512, (tt + 1) * 512)
                            if moe:
                                emit_norm(acc[:, :, tsl], "facc%d" % tt, lambda dc: nffn[:, l, dc:dc + 1], hT[:, :, tsl], "fhT",
                                          sq[:], "fsq", rstd[:], "frstd", h32=sq[:], h32k="fsq")
                                ps, pk = nps()
                                for s in range(4):
                                    for kc in range(8):
                                        P.add("pe", lambda e, ps=ps, kc=kc, s=s: e.matmul(
                                            ps[:, s * NE:(s + 1) * NE], lhsT=sq[:, kc, s * 128:(s + 1) * 128], rhs=rw[:, kc, :],
                                            start=(kc == 0), stop=(kc == 7)), ["fsq", C], [pk])
                                f2 = lambda t: t[:].rearrange("p s e -> p (s e)")
                                bc = lambda v: v.unsqueeze(2).broadcast_to([128, 4, NE])
                                P.add("dve", lambda e, ps=ps: e.tensor_copy(out=f2(lg), in_=ps[:, 0:4 * NE]), [pk], ["flg"])
                                P.add("dve", lambda e: e.tensor_reduce(out=mx[:, 0, :], in_=lg[:], axis=AX.X, op=ALU.max),
                                      ["flg"], ["fmx"])
                                P.add("dve", lambda e: e.tensor_tensor(out=msk[:], in0=lg[:], in1=bc(mx[:, 0, :]), op=ALU.is_ge),
                                      ["flg", "fmx"], ["fmsk"])
                                P.add("dve", lambda e: e.scalar_tensor_tensor(out=f2(l2), in0=f2(msk), scalar=-1e30, in1=f2(lg),
                                                                              op0=ALU.mult, op1=ALU.add), ["fmsk", "flg"], ["fl2"])
                                P.add("dve", lambda e: e.tensor_reduce(out=mx[:, 1, :], in_=l2[:], axis=AX.X, op=ALU.max),
                                      ["fl2"], ["fmx"])
                                P.add("dve", lambda e: e.tensor_tensor(out=msk[:], in0=lg[:], in1=bc(mx[:, 1, :]), op=ALU.is_ge),
                                      ["flg", "fmx"], ["fmsk"])
                                P.add("dve", lambda e: e.tensor_tensor(out=gt[:], in0=lg[:], in1=bc(mx[:, 0, :]), op=ALU.subtract),
                                      ["flg", "fmx"], ["fgt"])
                                P.add("act", lambda e: e.activation(out=f2(gt), in_=f2(gt), func=AF.Exp), ["fgt"], ["fgt"])
                                P.add("dve", lambda e: e.tensor_mul(out=f2(gt), in0=f2(gt), in1=f2(msk)), ["fgt", "fmsk"], ["fgt"])
                                P.add("dve", lambda e: e.tensor_reduce(out=den[:], in_=gt[:], axis=AX.X, op=ALU.add),
                                      ["fgt"], ["fden"])
                                P.add("dve", lambda e: e.reciprocal(out=den[:], in_=den[:]), ["fden"], ["fden"])
                                P.add("dve", lambda e: e.tensor_tensor(out=gt[:], in0=gt[:], in1=bc(den[:]), op=ALU.mult),
                                      ["fgt", "fden"], ["fgt"])
                                for s in range(4):
                                    gx = gexp2[s % 2]
                                    gk = "fgexp%d" % (s % 2)
                                    P.add("dve", lambda e, s=s, gx=gx: e.tensor_copy(
                                        out=gx[:], in_=gt[:, s, :].unsqueeze(2).broadcast_to([128, NE, 128])),
                                        ["fgt"], [gk])
                                    ps, pk = nps()
                                    for ee in range(NE):
                                        P.add("pe", lambda e, ps=ps, ee=ee, gx=gx: e.matmul(
                                            ps[:, ee * 128:(ee + 1) * 128], lhsT=gx[:, ee, :], rhs=ident[:],
                                            start=True, stop=True), [gk, C], [pk])
                                    P.add("act", lambda e, ps=ps, tt=tt, s=s: e.copy(
                                        out=gbc[:, :, tt * 512 + s * 128:tt * 512 + (s + 1) * 128],
                                        in_=ps[:, :].rearrange("p (e t) -> p e t", e=NE)), [pk], ["fgbc"])
                            else:
                                emit_norm(acc[:, :, tsl], "facc%d" % tt, lambda dc: nffn[:, l, dc:dc + 1], hT[:, :, tsl], "fhT",
                                          sq[:], "fsq", rstd[:], "frstd")
                        NFG = DFF // 256
                        NTT = HALF // 512
                        groups = [(ex, fg) for ex in range(nexp) for fg in range(NFG)]
                        PA = [(PS[0], "ps0"), (PS[1], "ps1")]
                        PY = [(PS[2], "ps2"), (PS[3], "ps3")]

                        def ld_w(gi):
                            ex, fg = groups[gi]
                            wb = gi % 2
                            P.dma("pool", wt[wb][:], WB[ex, fg, :, :], [], ["fw%d" % wb], "fw%d" % wb)

                        def emit_ab(gi, tt, n):
                            ex, fg = groups[gi]
                            wb = gi % 2
                            tsl = slice(tt * 512, (tt + 1) * 512)
                            for fc in range(2):
                                pa, pak = PA[fc]
                                for kc in range(8):
                                    P.add("pe", lambda e, pa=pa, kc=kc, fc=fc, wb=wb, tsl=tsl: e.matmul(
                                        pa[:, 0:512], lhsT=wt[wb][:, kc * 256 + fc * 128:kc * 256 + (fc + 1) * 128],
                                        rhs=hT[:, kc, tsl], start=(kc == 0), stop=(kc == 7)), ["fhT", "fw%d" % wb], [pak])
                                for kc in range(8):
                                    P.add("pe", lambda e, pa=pa, kc=kc, fc=fc, wb=wb, tsl=tsl: e.matmul(
                                        pa[:, 512:1024], lhsT=wt[wb][:, 2048 + kc * 256 + fc * 128:2048 + kc * 256 + (fc + 1) * 128],
                                        rhs=hT[:, kc, tsl], start=(kc == 0), stop=(kc == 7)), ["fhT", "fw%d" % wb], [pak])
                                ai = 2 * (n % 2) + fc
                                P.add("act", lambda e, pa=pa, fc=fc: e.activation(out=sa[fc][:], in_=pa[:, 0:512], func=AF.Silu),
                                      [pak], ["fsa%d" % fc])
                                if moe:
                                    P.add("dve", lambda e, pa=pa, fc=fc: e.tensor_mul(out=pr[fc][:], in0=sa[fc][:], in1=pa[:, 512:1024]),
                                          [pak, "fsa%d" % fc], ["fpr%d" % fc])
                                    P.add("dve", lambda e, fc=fc, ai=ai, ex=ex, tsl=tsl: e.tensor_mul(
                                        out=act[ai][:], in0=pr[fc][:], in1=gbc[:, ex, tsl]), ["fpr%d" % fc, "fgbc"], ["fact%d" % ai])
                                else:
                                    P.add("dve", lambda e, pa=pa, fc=fc, ai=ai: e.tensor_mul(out=act[ai][:], in0=sa[fc][:], in1=pa[:, 512:1024]),
                                          [pak, "fsa%d" % fc], ["fact%d" % ai])

                        def emit_y(gi, tt, n):
                            wb = gi % 2
                            tsl = slice(tt * 512, (tt + 1) * 512)
                            for dh in range(4):
                                py, pyk = PY[dh % 2]
                                for d2 in range(2):
                                    dc = dh * 2 + d2
                                    for fc in range(2):
                                        ai = 2 * (n % 2) + fc
                                        P.add("pe", lambda e, py=py, d2=d2, dc=dc, fc=fc, wb=wb, ai=ai: e.matmul(
                                            py[:, d2 * 512:(d2 + 1) * 512],
                                            lhsT=wt[wb][:, 4096 + fc * 1024 + dc * 128:4096 + fc * 1024 + (dc + 1) * 128],
                                            rhs=act[ai][:], start=(fc == 0), stop=(fc == 1)),
                                            ["fw%d" % wb, "fact%d" % ai], [pyk])
                                P.add("dve", lambda e, py=py, dh=dh, tsl=tsl: e.tensor_add(
                                    out=acc[:, dh * 2:dh * 2 + 2, tsl], in0=acc[:, dh * 2:dh * 2 + 2, tsl],
                                    in1=py[:, :].rearrange("p (c t) -> p c t", c=2)), [pyk, "facc%d" % tt], ["facc%d" % tt])

                        ld_w(0)
                        if len(groups) > 1:
                            ld_w(1)
                        prev = None
                        n = 0
                        for gi in range(len(groups)):
                            for tt in range(NTT):
                                emit_ab(gi, tt, n)
                                if prev is not None:
                                    emit_y(*prev)
                                if tt == 0 and gi >= 1 and gi + 1 < len(groups):
                                    ld_w(gi + 1)
                                prev = (gi, tt, n)
                                n += 1
                        emit_y(*prev)
                        for tt in range(HALF // 512):
                            P.dma("pool", xT[:, :, t0 + tt * 512:t0 + (tt + 1) * 512].rearrange("c p t -> p c t"),
                                  acc[:, :, tt * 512:(tt + 1) * 512], ["facc%d" % tt], ["xT"], "faccst%d" % tt)
                P.barrier()
                chk("ffn_%d" % l)
                if dbg and ("xT_ffn%d" % l) in dbg_outs:
                    P.dma("sp", dbg_outs["xT_ffn%d" % l].rearrange("(c p) t -> c p t", p=128), xT[:, :, :], ["xT"], [], "dbg")
                    P.barrier()

            with ExitStack() as st:
                xt = [sb("z_xt%d" % i, [128, 8, 512], st=st) for i in range(2)]
                sq = sb("z_sq", [128, 8, 512], st=st)
                rstd = sb("z_rstd", [128, 512], st=st)
                hn2 = [sb("z_hn%d" % i, [128, 8, 512], st=st) for i in range(2)]
                yo = [sb("z_yo%d" % i, [128, 1024], st=st) for i in range(4)]

                def ld_z(j):
                    b = j % 2
                    P.dma("sp", xt[b][:], xT[:, :, j * 512:(j + 1) * 512].rearrange("c p t -> p c t"),
                          ["xT"], ["zxt%d" % b], "zxt%d" % b)
                ld_z(0)
                for j in range(NT512):
                    b = j % 2
                    if j + 1 < NT512:
                        ld_z(j + 1)
                    hn = hn2[b]
                    emit_norm(xt[b][:], "zxt%d" % b, lambda dc: fnw[:, dc:dc + 1], hn[:], "zhn%d" % b, sq[:], "zsq", rstd[:], "zrstd")
                    for s in range(4):
                        ob = (j * 4 + s) % 4
                        ps, pk = nps()
                        for dc in range(8):
                            P.add("pe", lambda e, ps=ps, dc=dc, s=s, hn=hn: e.transpose(
                                out=ps[:, dc * 128:(dc + 1) * 128], in_=hn[:, dc, s * 128:(s + 1) * 128], identity=ident[:]),
                                ["zhn%d" % b, C], [pk])
                        P.add("dve", lambda e, ps=ps, ob=ob: e.tensor_copy(out=yo[ob][:], in_=ps[:, :]), [pk], ["zyo%d" % ob])
                        r0 = j * 512 + s * 128
                        P.dma("pool", out_d[r0:r0 + 128, :], yo[ob][:], ["zyo%d" % ob], [], "zyo%d" % ob)
        except _Stop:
            P.barrier()
            P.dma("sp", dbg_outs["stop"].rearrange("(c p) t -> c p t", p=128), xT[:, :, :], ["xT"], [], "dbg")
        nsem = P.emit()
    return nc, nsem, len(P.ops)


def make_consts():
    ident = np.eye(128, dtype=np.float32)
    s = np.arange(128)[:, None]
    t = np.arange(128)[None, :]
    same = (s // 64) == (t // 64)
    triu = (same & (s <= t)).astype(np.float32)
    tris = (same & (s > t)).astype(np.float32)
    sl = (np.arange(128) % 64)[:, None]
    tt = np.arange(64)[None, :]
    m = (sl <= tt).astype(np.float32)
    mask = np.tile(m[:, None, :], (1, 8, 1)).reshape(128, 512)
    return ident, triu, tris, np.ascontiguousarray(mask)


def ffn_blob(w1, w3, w2):
    w1 = np.asarray(w1, dtype=np.float32)
    w3 = np.asarray(w3, dtype=np.float32)
    w2 = np.asarray(w2, dtype=np.float32)
    ne = w1.shape[0]
    nfg = DFF // 256
    a = w1.reshape(ne, 8, 128, nfg, 256).transpose(0, 3, 2, 1, 4).reshape(ne, nfg, 128, 2048)
    b = w3.reshape(ne, 8, 128, nfg, 256).transpose(0, 3, 2, 1, 4).reshape(ne, nfg, 128, 2048)
    c = w2.reshape(ne, nfg, 2, 128, D).transpose(0, 1, 3, 2, 4).reshape(ne, nfg, 128, 2048)
    return np.ascontiguousarray(np.concatenate([a, b, c], axis=-1))


def layout_inputs(inp, seq, nt):
    f = lambda a: np.ascontiguousarray(np.asarray(a, dtype=np.float32))
    ident, triu, tris, mask = make_consts()
    shared = {
        "w_in": f(inp["w_in"]),
        "lower_bounds": f(inp["lower_bounds"]),
        "hgrn_norm_w": f(inp["hgrn_norm_w"]).reshape(DEPTH, 128, 1),
        "conv_w": f(np.asarray(inp["conv_w"]).reshape(DEPTH, 3, 8, 128).transpose(0, 3, 1, 2).reshape(DEPTH, 128, 24)),
        "w_proj_hgrn": f(inp["w_proj_hgrn"]),
        "w_proj_conv": f(inp["w_proj_conv"]),
        "w_out": f(inp["w_out"]),
        "norm_mix": f(np.asarray(inp["norm_mix"]).reshape(DEPTH, 8, 128).transpose(0, 2, 1)),
        "norm_ffn": f(np.asarray(inp["norm_ffn"]).reshape(DEPTH, 8, 128).transpose(0, 2, 1)),
        "dense_blob": ffn_blob(inp["dense_w1"], inp["dense_w3"], inp["dense_w2"]),
        "router_w": f(np.asarray(inp["router_w"]).reshape(8, 128, NE).transpose(1, 0, 2)),
        "expert_blob": ffn_blob(np.asarray(inp["expert_w1"])[0], np.asarray(inp["expert_w3"])[0],
                                np.asarray(inp["expert_w2"])[0]),
        "final_norm": f(np.asarray(inp["final_norm"]).reshape(8, 128).T),
        "c_ident": ident, "c_triu": triu, "c_tris": tris, "c_mask": mask,
    }
    x = np.asarray(inp["x"], dtype=np.float32)
    maps = []
    for c in range(NCORES):
        b, hf = c // 2, c % 2
        m = dict(shared)
        m["x"] = np.ascontiguousarray(x[b, hf * nt:(hf + 1) * nt, :])
        m["c_sel"] = np.full((128, 1), float(hf), dtype=np.float32)
        maps.append(m)
    return maps


_CACHE = {}


def run(inputs, dbg=None, trace=False):
    x = np.asarray(inputs["x"])
    bsz, seq, _ = x.shape
    assert bsz * 2 == NCORES
    nt = seq // 2
    key = (nt, tuple(sorted(dbg.items())) if dbg else None)
    if key not in _CACHE:
        _CACHE[key] = build_program(nt, dbg)[0]
    nc = _CACHE[key]
    maps = layout_inputs(inputs, seq, nt)
    res = run_bass_kernel_spmd(nc, maps, core_ids=list(range(NCORES)), **({"trace": True} if trace else {}))
    out = np.empty((bsz, seq, D), dtype=np.float32)
    for c in range(NCORES):
        b, hf = c // 2, c % 2
        out[b, hf * nt:(hf + 1) * nt, :] = res.results[c]["out"]
    return out, res


def kernel(**inputs):
    out, _ = run(inputs)
    return out
```
